# Optimizing a Trainium2 kernel written in Bass

```python
import jax
import jax.numpy as jnp
from jax import lax
import numpy as np

D_MODEL = 2048
BATCH = 1
SEQ = 8192
DEPTH = 4

ATT_HEADS = 8
ATT_HEAD_DIM = 128
ATT_WIDTH = ATT_HEADS * ATT_HEAD_DIM
ROPE_DIM = ATT_HEAD_DIM // 4
ROPE_THETA = 500000.0
MOBA_BLOCK = 256
MOBA_TOPK = 3
MOBA_Q_CHUNK = 64

RET_HEADS = 8
RET_QK_DIM = 64
RET_V_DIM = 128
RET_QK_WIDTH = RET_HEADS * RET_QK_DIM
RET_V_WIDTH = RET_HEADS * RET_V_DIM
RET_CHUNK = 256
RET_ROPE_THETA = 10000.0

POOL_WINDOWS = (2, 4, 8, 16)
POOL_GROUPS = 4
POOL_GROUP_DIM = 256
POOL_WIDTH = POOL_GROUPS * POOL_GROUP_DIM

N_BRANCH = 3
BRANCH_WIDTH = 1024
IN_SPLITS = (ATT_WIDTH, ATT_WIDTH, ATT_WIDTH, RET_QK_WIDTH, RET_QK_WIDTH,
             RET_V_WIDTH, RET_V_WIDTH, POOL_WIDTH, N_BRANCH * D_MODEL)
N_IN = sum(IN_SPLITS)

N_GROUPS = 4
EXPERTS_PER_GROUP = 8
N_EXPERTS = N_GROUPS * EXPERTS_PER_GROUP
TOP_K = 2
D_EXPERT = 512
MOE_BLOCK = 256

DN_ALPHA = (2 * DEPTH) ** 0.25
DN_BETA = (8 * DEPTH) ** -0.25
LN_EPS = 1e-5
NEG = -1e30

kernel_name = "hybrid_moba_retention_pool_hmoe"


def layer_norm(x, g, b):
    xf = x.astype(jnp.float32)
    mu = jnp.mean(xf, axis=-1, keepdims=True)
    var = jnp.mean(jnp.square(xf - mu), axis=-1, keepdims=True)
    return ((xf - mu) * lax.rsqrt(var + LN_EPS) * g + b).astype(x.dtype)


def split_heads(a, n_heads):
    b, s, w = a.shape
    return a.reshape(b, s, n_heads, w // n_heads).transpose(0, 2, 1, 3)


def merge_heads(a):
    b, h, s, d = a.shape
    return a.transpose(0, 2, 1, 3).reshape(b, s, h * d)


def rotary(x, rot_dim, theta):
    s = x.shape[2]
    half = rot_dim // 2
    inv_freq = 1.0 / (theta ** (jnp.arange(0, rot_dim, 2, dtype=jnp.float32) / rot_dim))
    ang = jnp.arange(s, dtype=jnp.float32)[:, None] * inv_freq[None, :]
    cos, sin = jnp.cos(ang), jnp.sin(ang)
    xr = x[..., :rot_dim].astype(jnp.float32)
    x1, x2 = xr[..., :half], xr[..., half:]
    rot = jnp.concatenate([x1 * cos - x2 * sin, x1 * sin + x2 * cos], axis=-1).astype(x.dtype)
    return jnp.concatenate([rot, x[..., rot_dim:]], axis=-1)


def moba_attention(q, k, v):
    b, h, s, hd = q.shape
    nb = -(-s // MOBA_BLOCK)
    sp = nb * MOBA_BLOCK
    pad = ((0, 0), (0, 0), (0, sp - s), (0, 0))
    q, k, v = jnp.pad(q, pad), jnp.pad(k, pad), jnp.pad(v, pad)
    kb = k.reshape(b, h, nb, MOBA_BLOCK, hd)
    vb = v.reshape(b, h, nb, MOBA_BLOCK, hd)
    kmean = jnp.mean(kb.astype(jnp.float32), axis=3)
    gate = jnp.einsum('bhsd,bhnd->bhsn', q.astype(jnp.float32), kmean)
    q_blk = jnp.arange(sp) // MOBA_BLOCK
    past = jnp.arange(nb)[None, :] < q_blk[:, None]
    gate = jnp.where(past, gate, NEG)
    k_sel = min(MOBA_TOPK, nb)
    _, sel = lax.top_k(gate, k_sel)
    sel_valid = sel < q_blk[:, None]

    nc = sp // MOBA_Q_CHUNK

    def to_chunks(a):
        a = a.reshape(b, h, nc, MOBA_Q_CHUNK, *a.shape[3:])
        return jnp.moveaxis(a, 2, 0)

    scale = hd ** -0.5
    b_idx = jnp.arange(b)[:, None, None, None]
    h_idx = jnp.arange(h)[None, :, None, None]
    key_off = jnp.arange(MOBA_BLOCK)

    def chunk_fn(args):
        qc, selc, validc, c = args
        t = c * MOBA_Q_CHUNK + jnp.arange(MOBA_Q_CHUNK)
        own = (c * MOBA_Q_CHUNK) // MOBA_BLOCK
        k_g = kb[b_idx, h_idx, selc]
        v_g = vb[b_idx, h_idx, selc]
        s_sel = jnp.einsum('bhqd,bhqkpd->bhqkp', qc, k_g,
                           preferred_element_type=jnp.float32) * scale
        s_sel = jnp.where(validc[..., None], s_sel, NEG)
        s_sel = s_sel.reshape(b, h, MOBA_Q_CHUNK, k_sel * MOBA_BLOCK)
        k_own = lax.dynamic_index_in_dim(kb, own, axis=2, keepdims=False)
        v_own = lax.dynamic_index_in_dim(vb, own, axis=2, keepdims=False)
        s_own = jnp.einsum('bhqd,bhpd->bhqp', qc, k_own,
                           preferred_element_type=jnp.float32) * scale
        causal = (own * MOBA_BLOCK + key_off)[None, :] <= t[:, None]
        s_own = jnp.where(causal, s_own, NEG)
        p = jax.nn.softmax(jnp.concatenate([s_sel, s_own], axis=-1), axis=-1)
        p_sel = p[..., :k_sel * MOBA_BLOCK].reshape(b, h, MOBA_Q_CHUNK, k_sel, MOBA_BLOCK).astype(v.dtype)
        p_own = p[..., k_sel * MOBA_BLOCK:].astype(v.dtype)
        return (jnp.einsum('bhqkp,bhqkpd->bhqd', p_sel, v_g)
                + jnp.einsum('bhqp,bhpd->bhqd', p_own, v_own))

    out = lax.map(chunk_fn, (to_chunks(q), to_chunks(sel), to_chunks(sel_valid),
                             jnp.arange(nc)))
    out = jnp.moveaxis(out, 0, 2).reshape(b, h, sp, hd)
    return out[:, :, :s]


def retention(q, k, v):
    b, h, s, dk = q.shape
    dv = v.shape[-1]
    n = -(-s // RET_CHUNK)
    sp = n * RET_CHUNK
    c = RET_CHUNK
    pad = ((0, 0), (0, 0), (0, sp - s), (0, 0))
    q = jnp.pad(q, pad).astype(jnp.float32).reshape(b, h, n, c, dk)
    k = jnp.pad(k, pad).astype(jnp.float32).reshape(b, h, n, c, dk) * (dk ** -0.5)
    v = jnp.pad(v, pad).astype(jnp.float32).reshape(b, h, n, c, dv)
    log_gamma = jnp.log1p(-jnp.exp2(-5.0 - jnp.arange(h, dtype=jnp.float32)))
    pos = jnp.arange(c, dtype=jnp.float32)
    diff = pos[:, None] - pos[None, :]
    decay = jnp.where(diff >= 0, jnp.exp(log_gamma[:, None, None] * jnp.maximum(diff, 0.0)), 0.0)
    inner = jnp.einsum('bhncd,bhnmd->bhncm', q, k) * decay[None, :, None]
    inner = jnp.einsum('bhncm,bhnme->bhnce', inner, v)
    k_decay = jnp.exp(log_gamma[:, None] * (c - 1.0 - pos)[None, :])
    q_decay = jnp.exp(log_gamma[:, None] * (pos + 1.0)[None, :])
    chunk_decay = jnp.exp(log_gamma * c)[None, :, None, None]
    kv = jnp.einsum('bhncd,bhnce->bhnde', k * k_decay[None, :, None, :, None], v)

    def step(state, kv_i):
        return state * chunk_decay + kv_i, state

    _, prev = lax.scan(step, jnp.zeros((b, h, dk, dv), jnp.float32), jnp.moveaxis(kv, 2, 0))
    prev = jnp.moveaxis(prev, 0, 2)
    cross = jnp.einsum('bhncd,bhnde->bhnce', q * q_decay[None, :, None, :, None], prev)
    return (inner + cross).reshape(b, h, sp, dv)[:, :, :s]


def multi_scale_pool(p, w_pool, pool_scale):
    b, s, _ = p.shape
    pf = p.astype(jnp.float32)
    cs = jnp.cumsum(pf, axis=1)
    n_avail = jnp.arange(1, s + 1, dtype=jnp.float32)
    outs = []
    for gi, w in enumerate(POOL_WINDOWS):
        lo, hi = gi * POOL_GROUP_DIM, (gi + 1) * POOL_GROUP_DIM
        cg = cs[..., lo:hi]
        lower = jnp.pad(cg, ((0, 0), (w, 0), (0, 0)))[:, :s]
        mean = (cg - lower) / jnp.minimum(n_avail, float(w))[None, :, None]
        outs.append(mean - pf[..., lo:hi])
    d = jnp.stack(outs, axis=2)
    y = jnp.einsum('bsgc,gce->bsge', d, w_pool).reshape(b, s, POOL_WIDTH)
    return (y * pool_scale).astype(p.dtype)


def token_mixer(x, w_in, ret_gain, w_pool, pool_scale, w_branch, w_out):
    b, s, _ = x.shape
    offsets = np.cumsum(IN_SPLITS)[:-1].tolist()
    q_a, k_a, v_a, q_r, k_r, v_r, g_r, p_in, gate_logits = jnp.split(x @ w_in, offsets, axis=-1)
    q_a = rotary(split_heads(q_a, ATT_HEADS), ROPE_DIM, ROPE_THETA)
    k_a = rotary(split_heads(k_a, ATT_HEADS), ROPE_DIM, ROPE_THETA)
    y_a = merge_heads(moba_attention(q_a, k_a, split_heads(v_a, ATT_HEADS)))
    q_r = rotary(split_heads(q_r, RET_HEADS), RET_QK_DIM, RET_ROPE_THETA)
    k_r = rotary(split_heads(k_r, RET_HEADS), RET_QK_DIM, RET_ROPE_THETA)
    r = retention(q_r, k_r, split_heads(v_r, RET_HEADS))
    mu = jnp.mean(r, axis=-1, keepdims=True)
    var = jnp.mean(jnp.square(r - mu), axis=-1, keepdims=True)
    r = merge_heads((r - mu) * lax.rsqrt(var + LN_EPS)) * ret_gain
    y_r = (jax.nn.silu(g_r.astype(jnp.float32)) * r).astype(x.dtype)
    y_p = multi_scale_pool(p_in, w_pool, pool_scale)
    branches = jnp.stack([y_a, y_r, y_p], axis=2)
    y_br = jnp.einsum('bsnc,ncd->bsnd', branches, w_branch)
    gates = jax.nn.sigmoid(gate_logits.reshape(b, s, N_BRANCH, D_MODEL))
    merged = jnp.sum(gates * y_br, axis=2)
    return merged @ w_out


def routed_experts(h, expert, weight, w_gate, w_up, w_down):
    t, d = h.shape
    a = t * TOP_K
    flat_e = expert.reshape(a)
    order = jnp.argsort(flat_e)
    e_sorted = flat_e[order]
    tok_sorted = order // TOP_K
    w_sorted = weight.reshape(a)[order]
    counts = jnp.bincount(flat_e, length=N_EXPERTS)
    padded = (counts + MOE_BLOCK - 1) // MOE_BLOCK * MOE_BLOCK
    pad_end = jnp.cumsum(padded)
    pad_start = pad_end - padded
    start = jnp.cumsum(counts) - counts
    slot = pad_start[e_sorted] + jnp.arange(a) - start[e_sorted]
    n_blocks = -(-a // MOE_BLOCK) + N_EXPERTS
    n_slots = n_blocks * MOE_BLOCK
    slot_tok = jnp.zeros((n_slots,), jnp.int32).at[slot].set(tok_sorted.astype(jnp.int32))
    block_expert = jnp.minimum(
        jnp.searchsorted(pad_end, jnp.arange(n_blocks) * MOE_BLOCK, side='right'), N_EXPERTS - 1)
    xb = h[slot_tok].reshape(n_blocks, MOE_BLOCK, d)

    def expert_block(args):
        xe, e = args
        return (jax.nn.silu(xe @ w_gate[e]) * (xe @ w_up[e])) @ w_down[e]

    yb = lax.map(expert_block, (xb, block_expert)).reshape(n_slots, d)
    y_assign = yb[slot] * w_sorted[:, None].astype(yb.dtype)
    return jnp.zeros((t, d), yb.dtype).at[tok_sorted].add(y_assign)


def hierarchical_moe(x, w_r1, b_r1, w_r2, b_r2, w_e_gate, w_e_up, w_e_down):
    b, s, d = x.shape
    h = x.reshape(b * s, d)
    t = h.shape[0]
    p1 = jax.nn.softmax((h @ w_r1).astype(jnp.float32) + b_r1, axis=-1)
    g_top, g_idx = lax.top_k(p1, 1)
    logits2 = ((h @ w_r2).astype(jnp.float32) + b_r2).reshape(t, N_GROUPS, EXPERTS_PER_GROUP)
    logits2 = jnp.take_along_axis(logits2, g_idx[:, :, None], axis=1)[:, 0]
    p2 = jax.nn.softmax(logits2, axis=-1)
    e_top, e_idx = lax.top_k(p2, TOP_K)
    weight = g_top * e_top / jnp.sum(e_top, axis=-1, keepdims=True)
    expert = g_idx * EXPERTS_PER_GROUP + e_idx
    y = routed_experts(h, expert, weight, w_e_gate, w_e_up, w_e_down)
    return y.reshape(b, s, d).astype(x.dtype)


def setup_inputs(seed: int = 0) -> dict:
    key = jax.random.key(seed)
    ks = jax.random.split(key, 20)
    f32 = jnp.float32
    L, D = DEPTH, D_MODEL

    def nrm(k, shape, scale):
        return jax.random.normal(k, shape, f32) * scale

    return {
        "x": nrm(ks[0], (BATCH, SEQ, D), 1.0),
        "w_in": nrm(ks[1], (L, D, N_IN), D ** -0.5),
        "ret_gain": 1.0 + nrm(ks[2], (L, RET_V_WIDTH), 0.02),
        "w_pool": nrm(ks[3], (L, POOL_GROUPS, POOL_GROUP_DIM, POOL_GROUP_DIM), POOL_GROUP_DIM ** -0.5),
        "pool_scale": 1.0 + nrm(ks[4], (L, POOL_WIDTH), 0.02),
        "w_branch": nrm(ks[5], (L, N_BRANCH, BRANCH_WIDTH, D), BRANCH_WIDTH ** -0.5),
        "w_out": nrm(ks[6], (L, D, D), DN_BETA * D ** -0.5),
        "ln1_g": 1.0 + nrm(ks[7], (L, D), 0.02),
        "ln1_b": nrm(ks[8], (L, D), 0.02),
        "w_r1": nrm(ks[9], (L, D, N_GROUPS), D ** -0.5),
        "b_r1": nrm(ks[10], (L, N_GROUPS), 0.01),
        "w_r2": nrm(ks[11], (L, D, N_EXPERTS), D ** -0.5),
        "b_r2": nrm(ks[12], (L, N_EXPERTS), 0.01),
        "w_e_gate": nrm(ks[13], (L, N_EXPERTS, D, D_EXPERT), D ** -0.5),
        "w_e_up": nrm(ks[14], (L, N_EXPERTS, D, D_EXPERT), D ** -0.5),
        "w_e_down": nrm(ks[15], (L, N_EXPERTS, D_EXPERT, D), DN_BETA * D_EXPERT ** -0.5),
        "ln2_g": 1.0 + nrm(ks[16], (L, D), 0.02),
        "ln2_b": nrm(ks[17], (L, D), 0.02),
    }


def reference(x, w_in, ret_gain, w_pool, pool_scale, w_branch, w_out, ln1_g, ln1_b,
              w_r1, b_r1, w_r2, b_r2, w_e_gate, w_e_up, w_e_down, ln2_g, ln2_b):
    for l in range(DEPTH):
        mix = token_mixer(x, w_in[l], ret_gain[l], w_pool[l], pool_scale[l], w_branch[l], w_out[l])
        x = layer_norm(DN_ALPHA * x + mix, ln1_g[l], ln1_b[l])
        ffn = hierarchical_moe(x, w_r1[l], b_r1[l], w_r2[l], b_r2[l],
                               w_e_gate[l], w_e_up[l], w_e_down[l])
        x = layer_norm(DN_ALPHA * x + ffn, ln2_g[l], ln2_b[l])
    return x
```

```python
import contextlib
import math
import numpy as np
import ml_dtypes
import concourse.bass as bass
import concourse.mybir as mybir
from concourse.bass_utils import run_bass_kernel_spmd
from concourse.alu_op_type import AluOpType as ALU

AF = mybir.ActivationFunctionType
AX = mybir.AxisListType
F32 = mybir.dt.float32
BF16 = mybir.dt.bfloat16
NPBF = ml_dtypes.bfloat16

NCORES = 8
D = 2048
SEQ = 8192
T = 1024
NTT = 8
NDC = 16
DEPTH = 4
N_IN = 13312
COL = dict(q_a=0, k_a=1024, v_a=2048, q_r=3072, k_r=3584, v_r=4096, g_r=5120, p_in=6144, gate=7168)
ALPHA = (2 * DEPTH) ** 0.25
LN_EPS = 1e-5
MASKV = 30000.0
ATT_SCALE = 128 ** -0.5
NEGBIG = -1e30


class Buf:
    __slots__ = ("name", "w", "r", "excl")

    def __init__(self, name, excl=False):
        self.name = name
        self.w = None
        self.r = []
        self.excl = excl


class Sched:
    def __init__(self, nc, stack):
        self.nc = nc
        self.stack = stack
        self.engs = {"pe": nc.tensor, "act": nc.scalar, "dve": nc.vector,
                     "pool": nc.gpsimd, "sp": nc.sync}
        self.sems = {}
        self.cnt = {}
        for k in self.engs:
            self.sems[k] = stack.enter_context(nc.semaphore("s_" + k))
            self.cnt[k] = 0
        self.waited = {k: {} for k in self.engs}
        self.n_wait = 0
        self.n_ins = 0

    def dma_sem(self, name):
        key = "dma_" + name
        self.sems[key] = self.stack.enter_context(self.nc.semaphore(key))
        self.cnt[key] = 0
        return key

    def _wait(self, eng, ev):
        if ev is None:
            return
        key, val = ev[0], ev[1]
        if self.waited[eng].get(key, 0) >= val:
            return
        self.engs[eng].wait_ge(self.sems[key], val)
        self.waited[eng][key] = val
        self.n_wait += 1

    def _deps(self, eng, reads, writes, pe_accum=False):
        for b in reads:
            self._wait(eng, b.w)
            if b.excl:
                for ev in b.r:
                    if ev[2] != eng:
                        self._wait(eng, ev)
        for b in writes:
            if not (pe_accum and b.w is not None and b.w[2] == "pe" and eng == "pe"):
                self._wait(eng, b.w)
            for ev in b.r:
                self._wait(eng, ev)

    def _commit(self, ev, reads, writes):
        for b in reads:
            b.r.append(ev)
            if len(b.r) > 16:
                best = {}
                for e in b.r:
                    if e[0] not in best or best[e[0]][1] < e[1]:
                        best[e[0]] = e
                b.r = list(best.values())
        for b in writes:
            b.w = ev
            b.r = []

    def op(self, eng, fn, *args, reads=(), writes=(), pe_accum=False, **kw):
        self._deps(eng, reads, writes, pe_accum)
        ins = fn(*args, **kw)
        self.cnt[eng] += 1
        ins.then_inc(self.sems[eng], 1)
        self._commit((eng, self.cnt[eng], eng), reads, writes)
        self.n_ins += 1
        return ins

    def dma(self, q, semkey, out, in_, reads=(), writes=(), **kw):
        self._deps(q, reads, writes)
        ins = self.engs[q].dma_start(out=out, in_=in_, **kw)
        self.cnt[semkey] += 16
        ins.then_inc(self.sems[semkey], 16)
        self._commit((semkey, self.cnt[semkey], "dma"), reads, writes)
        self.n_ins += 1
        return ins

    def barrier(self):
        for e in self.engs:
            for k in self.sems:
                if k != e and self.cnt[k] > 0:
                    self._wait(e, (k, self.cnt[k], None))


class Mem:
    def __init__(self, nc, stack, kb=204):
        self.words = kb * 256
        self.M = stack.enter_context(nc.sbuf_tensor("M", [128, self.words], F32))
        self.top = 0
        self.peak = 0

    def mark(self):
        return self.top

    def release(self, m):
        self.top = m

    def alloc(self, name, free_shape, dtype, parts=128):
        n = int(np.prod(free_shape))
        nwords = (n * (2 if dtype == BF16 else 4) + 3) // 4
        off = self.top
        self.top += (nwords + 15) // 16 * 16
        assert self.top <= self.words, f"SBUF overflow at {name}: {self.top * 4} B"
        self.peak = max(self.peak, self.top)
        v = self.M[0:parts, off:off + nwords]
        if dtype == BF16:
            v = v.bitcast(BF16)
        if len(free_shape) > 1:
            names = [f"a{i}" for i in range(len(free_shape))]
            pat = "p (" + " ".join(names) + ") -> p " + " ".join(names)
            v = v.rearrange(pat, **{nm: int(s) for nm, s in zip(names[:-1], free_shape[:-1])})
        return v, Buf(name)


def _mem_alloc_top(self, name, free_shape, dtype, parts=128):
    n = int(np.prod(free_shape))
    nwords = (n * (2 if dtype == BF16 else 4) + 3) // 4
    off = self.words - (nwords + 15) // 16 * 16
    v = self.M[0:parts, off:off + nwords]
    if dtype == BF16:
        v = v.bitcast(BF16)
    if len(free_shape) > 1:
        names = [f"a{i}" for i in range(len(free_shape))]
        pat = "p (" + " ".join(names) + ") -> p " + " ".join(names)
        v = v.rearrange(pat, **{nm: int(s) for nm, s in zip(names[:-1], free_shape[:-1])})
    self.top_limit = off
    return v, Buf(name)


Mem.alloc_top = _mem_alloc_top


class Ring:
    def __init__(self, items):
        self.items = items
        self.i = 0

    def next(self):
        it = self.items[self.i % len(self.items)]
        self.i += 1
        return it


def _gammas():
    return 1.0 - np.exp2(-5.0 - np.arange(8, dtype=np.float64))


def host_tables(core):
    f32 = np.float32
    tb = {}
    pos = core * T + np.arange(T, dtype=np.float32)
    invA = (1.0 / (np.float32(500000.0) ** (np.arange(0, 32, 2, dtype=np.float32) / np.float32(32)))).astype(f32)
    invR = (1.0 / (np.float32(10000.0) ** (np.arange(0, 64, 2, dtype=np.float32) / np.float32(64)))).astype(f32)
    angA = (pos[:, None] * invA[None, :]).astype(f32)
    angR = (pos[:, None] * invR[None, :]).astype(f32)
    rope = np.concatenate([np.cos(angA), np.sin(angA), np.cos(angR), np.sin(angR)], axis=1).astype(f32)
    tb["rope"] = np.ascontiguousarray(rope.reshape(NTT, 128, 96).transpose(1, 0, 2)).reshape(128, NTT * 96)
    g = np.zeros((128, 2, 4, 32), f32)
    for j in range(4):
        gb = core * 4 + j
        g[:, 0, j, :] = np.where(np.arange(32) < gb, 0.0, NEGBIG)
        g[:, 1, j, :] = np.where(np.arange(32) < gb, 1.0, 0.0)
    tb["gconst"] = g.reshape(128, 256)
    oh = np.zeros((32, 32, 128), f32)
    for b in range(32):
        oh[b, b, :] = 1.0
    tb["onehot"] = oh.reshape(32, 32 * 128)
    kk = np.arange(128)
    tb["tri"] = np.where(kk[:, None] <= kk[None, :], 0.0, -MASKV).astype(f32)
    tb["ident"] = np.eye(128, dtype=f32)
    gam = _gammas()
    lg = np.log(gam)
    p256 = np.arange(256, dtype=np.float64)
    dec = np.zeros((128, 8, 2, 256), np.float64)
    for h in range(8):
        for mc in range(2):
            m = mc * 128 + np.arange(128)
            diff = p256[None, :] - m[:, None]
            dec[:, h, mc, :] = np.where(diff >= 0, np.exp(lg[h] * np.maximum(diff, 0.0)), 0.0)
    tb["decT"] = dec.astype(f32).reshape(128, 8 * 2 * 256)
    qd = np.zeros((128, 4, 256), np.float64)
    for hp in range(4):
        for par in range(2):
            qd[par * 64:(par + 1) * 64, hp, :] = np.exp(lg[2 * hp + par] * (p256 + 1.0))[None, :]
    tb["qdec"] = qd.astype(f32).reshape(128, 1024)
    kd = np.zeros((128, 2, 8), np.float64)
    for ti in range(2):
        p = ti * 128 + np.arange(128)
        for h in range(8):
            kd[:, ti, h] = np.exp(lg[h] * (255.0 - p))
    tb["kdec"] = kd.astype(f32).reshape(128, 16)
    cd = np.zeros((128, 4), np.float64)
    sc = np.zeros((128, 4, 4), np.float64)
    co = np.zeros((128, 8, 4), np.float64)
    for hp in range(4):
        for par in range(2):
            h = 2 * hp + par
            sl = slice(par * 64, (par + 1) * 64)
            cd[sl, hp] = np.exp(lg[h] * 256.0)
            for i in range(4):
                sc[sl, i, hp] = np.exp(lg[h] * 256.0 * (3 - i))
            for c2 in range(8):
                co[sl, c2, hp] = np.exp(lg[h] * 1024.0 * (core - 1 - c2)) if c2 < core else 0.0
    tb["rsc"] = np.concatenate([cd.reshape(128, 4), sc.reshape(128, 16), co.reshape(128, 32)], axis=1).astype(f32)
    iv = np.zeros((128, 4, 16), f32)
    for gi, w in enumerate((2, 4, 8, 16)):
        n = np.minimum(core * T + np.arange(16) + 1.0, float(w))
        iv[:, gi, :] = (1.0 / n)[None, :]
    tb["invn"] = iv.reshape(128, 64)
    return tb


def feat_vec(v):
    return np.ascontiguousarray(v.reshape(-1, 128).T)


class Prog:
    def __init__(self, mode, dbg=None):
        self.mode = mode
        self.dbg = dbg
        self.nc = bass.Bass("TRN2", target_bir_lowering=False)
        self.ins = {}
        self.outs = {}

    def din(self, name, shape, dt=F32):
        self.ins[name] = (shape, dt)
        return self.nc.dram_tensor(name, list(shape), dt, kind="ExternalInput").ap()

    def dout(self, name, shape, dt=F32):
        self.outs[name] = (shape, dt)
        return self.nc.dram_tensor(name, list(shape), dt, kind="ExternalOutput").ap()

    def dscratch(self, name, shape, dt=F32):
        return self.nc.dram_tensor(name, list(shape), dt, kind="Internal").ap()


def build(mode, dbg=None):
    P = Prog(mode, dbg)
    nc = P.nc
    with contextlib.ExitStack() as st:
        S = Sched(nc, st)
        mem = Mem(nc, st)
        psb = [st.enter_context(nc.psum_tensor(f"ps{i}", [128, 512], F32)) for i in range(8)]
        psB = [Buf(f"ps{i}", excl=True) for i in range(8)]
        PS = [(psb[i][:], psB[i]) for i in range(8)]
        ctx = dict(P=P, nc=nc, S=S, mem=mem, PS=PS, stop=dbg)
        if mode == "A":
            prog_A(ctx)
        else:
            prog_L(ctx)
        S.barrier()
        P.stats = dict(ins=S.n_ins, waits=S.n_wait, peak_kb=mem.peak * 4 / 1024, cnt=dict(S.cnt))
    return P


def mm(S, nc, out, lhsT, rhs, start, stop, reads, writes, **kw):
    return S.op("pe", nc.tensor.matmul, out, lhsT, rhs, start=start, stop=stop,
                reads=reads, writes=writes, pe_accum=True, **kw)


def load_const(ctx, name, dram, shape, dtype, q="sp", parts=128):
    S, mem = ctx["S"], ctx["mem"]
    t, b = mem.alloc(name, shape[1:], dtype, parts=parts)
    sem = S.dma_sem(name)
    src = dram
    if len(shape) > 2:
        names = [f"a{i}" for i in range(len(shape) - 1)]
        pat = "p (" + " ".join(names) + ") -> p " + " ".join(names)
        src = dram.rearrange(pat, **{nm: int(s) for nm, s in zip(names[:-1], shape[1:-1])})
    S.dma(q, sem, t, src, writes=[b])
    return t, b


def inproj_tokmajor(ctx, xT, xTb, w_view, col0, ncols_grp, ngrp, wring, evac):
    S, nc, PS = ctx["S"], ctx["nc"], ctx["PS"]
    psr = ctx["psring"]
    pending = None
    for g in range(ngrp):
        wt, wb, wsem = wring.next()
        S.dma("pool", wsem, wt, w_view[:, :, col0 + g * ncols_grp: col0 + (g + 1) * ncols_grp], writes=[wb])
        for tt in range(NTT):
            ps, pb = psr.next()
            for dc in range(NDC):
                mm(S, nc, ps[:, 0:ncols_grp], xT[:, dc, tt * 128:(tt + 1) * 128], wt[:, dc, :],
                   dc == 0, dc == NDC - 1, [xTb, wb], [pb])
            if pending is not None:
                pending()
            pending = evac(g, tt, ps, pb)
    if pending is not None:
        pending()


def rotary_evac(ctx, ps, pb, dst, dstb, nh, hd, half, cos, sin, ropeb, tmp, tmpb, scale=None):
    S, nc = ctx["S"], ctx["nc"]
    psv = ps[:, 0:nh * hd].rearrange("p (h d) -> p h d", h=nh)
    dv = dst.rearrange("p (h d) -> p h d", h=nh)
    if scale is None:
        S.op("act", nc.scalar.copy, dst, ps[:, 0:nh * hd], reads=[pb], writes=[dstb])
    else:
        S.op("act", nc.scalar.mul, dst, ps[:, 0:nh * hd], scale, reads=[pb], writes=[dstb])
    cb = cos.unsqueeze(1).broadcast_to([128, nh, half])
    sb = sin.unsqueeze(1).broadcast_to([128, nh, half])
    x1 = psv[:, :, 0:half]
    x2 = psv[:, :, half:2 * half]
    t = [tmp[:, i, 0:nh * half].rearrange("p (h d) -> p h d", h=nh) for i in range(4)]
    S.op("dve", nc.vector.tensor_tensor, t[0], x1, cb, ALU.mult, reads=[pb, ropeb], writes=[tmpb[0]])
    S.op("dve", nc.vector.tensor_tensor, t[1], x2, sb, ALU.mult, reads=[pb, ropeb], writes=[tmpb[1]])
    S.op("dve", nc.vector.tensor_tensor, t[2], x1, sb, ALU.mult, reads=[pb, ropeb], writes=[tmpb[2]])
    S.op("dve", nc.vector.tensor_tensor, t[3], x2, cb, ALU.mult, reads=[pb, ropeb], writes=[tmpb[3]])
    if scale is None:
        S.op("dve", nc.vector.tensor_tensor, dv[:, :, 0:half], t[0], t[1], ALU.subtract,
             reads=[tmpb[0], tmpb[1]], writes=[dstb])
        S.op("dve", nc.vector.tensor_tensor, dv[:, :, half:2 * half], t[2], t[3], ALU.add,
             reads=[tmpb[2], tmpb[3]], writes=[dstb])
    else:
        S.op("dve", nc.vector.tensor_tensor, t[0], t[0], t[1], ALU.subtract,
             reads=[tmpb[0], tmpb[1]], writes=[tmpb[0]])
        S.op("dve", nc.vector.tensor_tensor, t[2], t[2], t[3], ALU.add,
             reads=[tmpb[2], tmpb[3]], writes=[tmpb[2]])
        S.op("act", nc.scalar.mul, dv[:, :, 0:half], t[0], scale, reads=[tmpb[0]], writes=[dstb])
        S.op("act", nc.scalar.mul, dv[:, :, half:2 * half], t[2], scale, reads=[tmpb[2]], writes=[dstb])


def common_setup(ctx, need):
    P, nc, S, mem = ctx["P"], ctx["nc"], ctx["S"], ctx["mem"]
    xT_d = P.din("xT", (D, T))
    ctx["xT_d"] = xT_d
    xT, xTb = mem.alloc("xT_bf", (NDC, T), BF16)
    sem = S.dma_sem("xT")
    xv = xT_d.rearrange("(dc p) t -> p dc t", p=128)
    for h in range(4):
        S.dma("pool", sem, xT[:, h * 4:(h + 1) * 4, :], xv[:, h * 4:(h + 1) * 4, :], writes=[xTb] if h == 3 else [])
    ctx["xT"], ctx["xTb"] = xT, xTb
    C = {}
    if "rope" in need:
        C["rope"] = load_const(ctx, "rope", P.din("rope", (128, NTT * 96)), (128, NTT, 96), F32)
    if "ident" in need:
        C["ident"] = load_const(ctx, "ident", P.din("ident", (128, 128)), (128, 128), BF16, q="pool")
    ctx["C"] = C


def transpose_to(ctx, src, srcb, ncol, dst_slices, dstb, identb):
    S, nc = ctx["S"], ctx["nc"]
    ident, ib = identb
    ps, pb = ctx["psring"].next()
    pst = ps[:, 0:256].bitcast(BF16)
    for k in range(ncol):
        S.op("pe", nc.tensor.transpose, pst[:, k * 128:(k + 1) * 128], src[:, k * 128:(k + 1) * 128], ident,
             reads=[srcb, ib], writes=[pb], pe_accum=True)
    return pst, pb


def prog_A(ctx):
    ctx["psring"] = Ring(ctx["PS"])
    common_setup(ctx, ["rope", "ident"])
    rsc, rscb = load_const(ctx, "rsc", ctx["P"].din("rsc", (128, 52)), (128, 52), F32)
    A_body(ctx, rsc, rscb, "")


def A_body(ctx, rsc, rscb, opre):
    P, nc, S, mem, PS = ctx["P"], ctx["nc"], ctx["S"], ctx["mem"], ctx["PS"]
    xT, xTb = ctx["xT"], ctx["xTb"]
    rope, ropeb = ctx["C"]["rope"]
    identb = ctx["C"]["ident"]
    w_a = P.din("w_a", (D, 4608))
    wv = w_a.rearrange("(dc p) n -> p dc n", p=128)
    kdec, kdecb = load_const(ctx, "kdec", P.din("kdec", (128, 16)), (128, 2, 8), F32)
    o_kT = P.dout(opre + "kT_loc", (1024, T), BF16)
    o_v = P.dout(opre + "v_loc", (8 * T, 128), BF16)
    o_kmean = P.dout(opre + "kmean", (128, 32))
    o_krT2 = P.dout(opre + "krT2", (128, 4 * T), BF16)
    o_vr = P.dout(opre + "vr_tok", (128, NTT * 1024), BF16)
    o_kv = P.dout(opre + "kv_loc", (128, 4 * 512))
    o_S = P.dout(opre + "S_loc", (128, 512))
    o_halo = P.dout(opre + "halo", (128, 8 * 16))

    kT, kTb = mem.alloc("kT", (8, T), BF16)
    krT2, krT2b = mem.alloc("krT2", (4, T), BF16)
    ktokd, ktokdb = mem.alloc("ktokd", (NTT, 512), BF16)
    vr, vrb = mem.alloc("vr", (NTT, 1024), BF16)
    wslots = []
    for i in range(2):
        t, b = mem.alloc(f"w{i}", (NDC, 512), BF16)
        wslots.append((t, b, S.dma_sem(f"w{i}")))
    wring = Ring(wslots)
    tokr = []
    for i in range(3):
        t, b = mem.alloc(f"tok{i}", (512,), BF16)
        tokr.append((t, b))
    tokring = Ring(tokr)
    tmp, _ = mem.alloc("rtmp", (4, 256), F32)
    tmpb = [Buf(f"rtmp{i}") for i in range(4)]
    vst = []
    for i in range(2):
        t, b = mem.alloc(f"vst{i}", (512,), BF16)
        vst.append((t, b))
    vring = Ring(vst)
    osem = S.dma_sem("outA")

    def evac_ka(g, tt, ps, pb):
        tk, tkb = tokring.next()
        rotary_evac(ctx, ps, pb, tk, tkb, 4, 128, 16, rope[:, tt, 0:16], rope[:, tt, 16:32], ropeb, tmp, tmpb)
        def post():
            pst, pstb = transpose_to(ctx, tk, tkb, 4, None, None, identb)
            S.op("act", nc.scalar.copy, kT[:, g * 4:(g + 1) * 4, tt * 128:(tt + 1) * 128],
                 pst.rearrange("p (h t) -> p h t", h=4), reads=[pstb], writes=[kTb])
        return post

    inproj_tokmajor(ctx, xT, xTb, wv, 0, 512, 2, wring, evac_ka)

    v_view = o_v.rearrange("(h t) d -> t h d", h=8)

    def evac_va(g, tt, ps, pb):
        t, b = vring.next()
        S.op("act", nc.scalar.copy, t, ps, reads=[pb], writes=[b])
        S.dma("sp", osem, v_view[tt * 128:(tt + 1) * 128, g * 4:(g + 1) * 4, :],
              t.rearrange("p (h d) -> p h d", h=4), reads=[b])

    inproj_tokmajor(ctx, xT, xTb, wv, 1024, 512, 2, wring, evac_va)

    S.dma("sp", osem, o_kT.rearrange("(h p) t -> p h t", p=128), kT, reads=[kTb])
    km, kmb = mem.alloc("km", (8, 4), F32)
    S.op("dve", nc.vector.tensor_reduce, km, kT.rearrange("p h (j t) -> p h j t", j=4), AX.X, ALU.add,
         reads=[kTb], writes=[kmb])
    S.op("act", nc.scalar.mul, km, km, 1.0 / 256.0, reads=[kmb], writes=[kmb])
    S.dma("sp", osem, o_kmean, km.rearrange("p h j -> p (h j)"), reads=[kmb])

    def evac_kr(g, tt, ps, pb):
        tk, tkb = tokring.next()
        rotary_evac(ctx, ps, pb, tk, tkb, 8, 64, 32, rope[:, tt, 32:64], rope[:, tt, 64:96], ropeb, tmp, tmpb,
                    scale=64 ** -0.5)
        S.op("dve", nc.vector.tensor_tensor, ktokd[:, tt, :].rearrange("p (h d) -> p h d", h=8),
             tk.rearrange("p (h d) -> p h d", h=8),
             kdec[:, tt % 2, :].unsqueeze(2).broadcast_to([128, 8, 64]), ALU.mult,
             reads=[tkb, kdecb], writes=[ktokdb])
        def post():
            pst, pstb = transpose_to(ctx, tk, tkb, 4, None, None, identb)
            S.op("act", nc.scalar.copy, krT2[:, :, tt * 128:(tt + 1) * 128],
                 pst.rearrange("p (h t) -> p h t", h=4), reads=[pstb], writes=[krT2b])
        return post

    inproj_tokmajor(ctx, xT, xTb, wv, 2048, 512, 1, wring, evac_kr)

    def evac_vr(g, tt, ps, pb):
        S.op("act", nc.scalar.copy, vr[:, tt, g * 512:(g + 1) * 512], ps, reads=[pb], writes=[vrb])

    inproj_tokmajor(ctx, xT, xTb, wv, 2560, 512, 2, wring, evac_vr)
    S.dma("sp", osem, o_krT2.rearrange("p (h t) -> p h t", h=4), krT2, reads=[krT2b])
    S.dma("sp", osem, o_vr.rearrange("p (a c) -> p a c", a=NTT), vr, reads=[vrb])

    kv, kvb = mem.alloc("kv", (4, 512), F32)
    Sl, Slb = mem.alloc("Sl", (512,), F32)
    for i in range(4):
        ps, pb = ctx["psring"].next()
        for h in range(8):
            hp, par = h // 2, h % 2
            for ti in range(2):
                tt = 2 * i + ti
                mm(S, nc, ps[par * 64:(par + 1) * 64, hp * 128:(hp + 1) * 128],
                   ktokd[:, tt, h * 64:(h + 1) * 64], vr[:, tt, h * 128:(h + 1) * 128],
                   ti == 0, ti == 1, [ktokdb, vrb], [pb], skip_group_check=True)
        S.op("act", nc.scalar.copy, kv[:, i, :], ps, reads=[pb], writes=[kvb])
        for hp in range(4):
            sl = slice(hp * 128, (hp + 1) * 128)
            if i == 0:
                S.op("dve", nc.vector.tensor_scalar_mul, Sl[:, sl], kv[:, i, sl], rsc[:, 4 + i * 4 + hp: 5 + i * 4 + hp],
                     reads=[kvb, rscb], writes=[Slb])
            else:
                S.op("dve", nc.vector.scalar_tensor_tensor, Sl[:, sl], kv[:, i, sl],
                     rsc[:, 4 + i * 4 + hp: 5 + i * 4 + hp], Sl[:, sl], ALU.mult, ALU.add,
                     reads=[kvb, rscb, Slb], writes=[Slb])
    S.dma("sp", osem, o_kv.rearrange("p (i c) -> p i c", i=4), kv, reads=[kvb])
    S.dma("sp", osem, o_S, Sl, reads=[Slb])

    hal, halb = mem.alloc("hal", (8, 16), F32)
    for g in range(2):
        wt, wb, wsem = wring.next()
        S.dma("pool", wsem, wt, wv[:, :, 3584 + g * 512: 3584 + (g + 1) * 512], writes=[wb])
        ps, pb = ctx["psring"].next()
        for cc in range(4):
            for dc in range(NDC):
                mm(S, nc, ps[:, cc * 16:(cc + 1) * 16], wt[:, dc, cc * 128:(cc + 1) * 128], xT[:, dc, T - 16:T],
                   dc == 0, dc == NDC - 1, [xTb, wb], [pb], skip_group_check=True)
        S.op("act", nc.scalar.copy, hal[:, g * 4:(g + 1) * 4, :].rearrange("p a b -> p (a b)"), ps[:, 0:64],
             reads=[pb], writes=[halb])
    S.dma("sp", osem, o_halo, hal.rearrange("p a b -> p (a b)"), reads=[halb])


WL = dict(q_a=0, q_r=1024, g_r=1536, p_in=2560, gate=3584)
WL_COLS = 9728


def wl_from_w_in(w_in_l):
    return np.ascontiguousarray(np.concatenate(
        [w_in_l[:, 0:1024], w_in_l[:, 3072:3584], w_in_l[:, 5120:6144], w_in_l[:, 6144:7168], w_in_l[:, 7168:13312]], axis=1))


def prog_L(ctx):
    P, nc, S, mem, PS = ctx["P"], ctx["nc"], ctx["S"], ctx["mem"], ctx["PS"]
    stop = ctx.get("stop")
    ctx["psring"] = Ring(PS)
    common_setup(ctx, ["rope", "ident"])
    xT, xTb = ctx["xT"], ctx["xTb"]
    xT_d = ctx["xT_d"]
    rope, ropeb = ctx["C"]["rope"]
    identb = ctx["C"]["ident"]
    ident, identB = identb
    w_l = P.din("w_l", (D, WL_COLS))
    wv = w_l.rearrange("(dc p) n -> p dc n", p=128)
    vecs, vecsb = load_const(ctx, "vecs", P.din("vecs", (128, 80)), (128, 80), F32)
    gain = vecs[:, 0:8]
    pscale = vecs[:, 8:16]
    ln1g, ln1b, ln2g, ln2b = vecs[:, 16:32], vecs[:, 32:48], vecs[:, 48:64], vecs[:, 64:80]
    rsc, rscb = load_const(ctx, "rsc", P.din("rsc", (128, 52)), (128, 52), F32)
    cdec = rsc[:, 0:4]
    coef = rsc[:, 20:52]
    epsv, epsb = mem.alloc("epsv", (1,), F32)
    S.op("dve", nc.vector.memset, epsv, LN_EPS, writes=[epsb])
    onesF, onesFb = mem.alloc("onesF", (128,), F32)
    S.op("dve", nc.vector.memset, onesF, 1.0, writes=[onesFb])
    yR_d = P.dout("yR", (1024, T), BF16)
    yA_d = P.dout("yA", (1024, T), BF16)
    yP_d = P.dout("yP", (1024, T), BF16)
    xo_d = P.dout("xT_out", (D, T))
    osem = S.dma_sem("outL")
    base_mark = mem.mark()

    def wslots_make(n, shape, nm):
        sl = []
        for i in range(n):
            t, b = mem.alloc(f"{nm}{i}", shape, BF16)
            sl.append((t, b, S.dma_sem(f"{nm}{i}")))
        return Ring(sl)

    qrT2, qrT2b = mem.alloc("qrT2", (4, T), BF16)
    qdT2, qdT2b = mem.alloc("qdT2", (4, T), BF16)
    krT2, krT2b = load_const(ctx, "krT2", P.din("krT2", (128, 4 * T), BF16), (128, 4, T), BF16)
    vr, vrb = load_const(ctx, "vr", P.din("vr_tok", (128, NTT * 1024), BF16), (128, NTT, 1024), BF16)
    decT, decTb = load_const(ctx, "decT", P.din("decT", (128, 4096)), (128, 8, 512), BF16, q="pool")
    qdec, qdecb = load_const(ctx, "qdec", P.din("qdec", (128, 1024)), (128, 4, 256), F32)
    kvs, kvsb = load_const(ctx, "kvs", P.din("kv_loc", (128, 2048)), (128, 4, 512), F32)
    Sall_d = P.din("S_all", (8 * 128, 512))
    Sall, Sallb = mem.alloc("Sall", (8, 512), F32)
    S.dma("sp", S.dma_sem("Sall"), Sall, Sall_d.rearrange("(c p) n -> p c n", p=128), writes=[Sallb])
    gs, gsb = mem.alloc("gs", (8, T), BF16)
    yR, yRb = mem.alloc("yR", (8, T), BF16)
    wring = wslots_make(2, (NDC, 512), "wr")
    tokr = Ring([mem.alloc(f"tok{i}", (512,), BF16) for i in range(3)])
    tmp, _ = mem.alloc("rtmp", (4, 256), F32)
    tmpb = [Buf(f"rtmp{i}") for i in range(4)]

    def evac_qr(g, tt, ps, pb):
        tk, tkb = tokr.next()
        rotary_evac(ctx, ps, pb, tk, tkb, 8, 64, 32, rope[:, tt, 32:64], rope[:, tt, 64:96], ropeb, tmp, tmpb)
        def post():
            pst, pstb = transpose_to(ctx, tk, tkb, 4, None, None, identb)
            S.op("act", nc.scalar.copy, qrT2[:, :, tt * 128:(tt + 1) * 128],
                 pst.rearrange("p (h t) -> p h t", h=4), reads=[pstb], writes=[qrT2b])
        return post

    inproj_tokmajor(ctx, xT, xTb, wv, WL["q_r"], 512, 1, wring, evac_qr)
    S.op("dve", nc.vector.tensor_tensor, qdT2.rearrange("p h (i c) -> p h i c", i=4),
         qrT2.rearrange("p h (i c) -> p h i c", i=4),
         qdec.unsqueeze(2).broadcast_to([128, 4, 4, 256]), ALU.mult,
         reads=[qrT2b, qdecb], writes=[qdT2b])
    for g in range(2):
        wt, wb, wsem = wring.next()
        S.dma("pool", wsem, wt, wv[:, :, WL["g_r"] + g * 512: WL["g_r"] + (g + 1) * 512], writes=[wb])
        for hh in range(4):
            for th in range(2):
                ps, pb = ctx["psring"].next()
                for dc in range(NDC):
                    mm(S, nc, ps, wt[:, dc, hh * 128:(hh + 1) * 128], xT[:, dc, th * 512:(th + 1) * 512],
                       dc == 0, dc == NDC - 1, [wb, xTb], [pb])
                S.op("act", nc.scalar.activation, gs[:, g * 4 + hh, th * 512:(th + 1) * 512], ps, AF.Silu,
                     reads=[pb], writes=[gsb])
    prev, prevb = mem.alloc("prev", (512,), F32)
    prevh, prevhb = mem.alloc("prevh", (512,), BF16)
    for c2 in range(8):
        for hp in range(4):
            sl = slice(hp * 128, (hp + 1) * 128)
            sc = coef[:, c2 * 4 + hp: c2 * 4 + hp + 1]
            if c2 == 0:
                S.op("dve", nc.vector.tensor_scalar_mul, prev[:, sl], Sall[:, c2, sl], sc,
                     reads=[Sallb, rscb], writes=[prevb])
            else:
                S.op("dve", nc.vector.scalar_tensor_tensor, prev[:, sl], Sall[:, c2, sl], sc, prev[:, sl],
                     ALU.mult, ALU.add, reads=[Sallb, rscb, prevb], writes=[prevb])
    adr = Ring([mem.alloc(f"adt{i}", (512,), BF16) for i in range(3)])
    f32r = Ring([mem.alloc(f"rf{i}", (256,), F32) for i in range(12)])
    for i in range(4):
        S.op("act", nc.scalar.copy, prevh, prev, reads=[prevb], writes=[prevhb])
        st = {}

        def stage1(h):
            hp, par = h // 2, h % 2
            pp = slice(par * 64, (par + 1) * 64)
            psA, psAb = ctx["psring"].next()
            for mc in range(2):
                mm(S, nc, psA[:, mc * 256:(mc + 1) * 256],
                   krT2[pp, hp, i * 256 + mc * 128: i * 256 + (mc + 1) * 128], qrT2[pp, hp, i * 256:(i + 1) * 256],
                   True, True, [krT2b, qrT2b], [psAb], skip_group_check=True)
            adt, adtb = adr.next()
            S.op("dve", nc.vector.tensor_tensor, adt, psA, decT[:, h, :], ALU.mult,
                 reads=[psAb, decTb], writes=[adtb])
            st[h] = dict(adt=adt, adtb=adtb)

        def stage2(h):
            hp, par = h // 2, h % 2
            pp = slice(par * 64, (par + 1) * 64)
            d = st[h]
            adt, adtb = d["adt"], d["adtb"]
            psR, psRb = ctx["psring"].next()
            mm(S, nc, psR[:, 0:256], vr[:, 2 * i, h * 128:(h + 1) * 128], adt[:, 0:256], True, False, [vrb, adtb], [psRb])
            mm(S, nc, psR[:, 0:256], vr[:, 2 * i + 1, h * 128:(h + 1) * 128], adt[:, 256:512], False, False, [vrb, adtb], [psRb])
            mm(S, nc, psR[:, 0:256], prevh[pp, hp * 128:(hp + 1) * 128], qdT2[pp, hp, i * 256:(i + 1) * 256],
               False, True, [prevhb, qdT2b], [psRb])
            r_sb, r_sbb = f32r.next()
            rsq, rsqb = f32r.next()
            S.op("act", nc.scalar.copy, r_sb, psR[:, 0:256], reads=[psRb], writes=[r_sbb])
            S.op("act", nc.scalar.activation, rsq, psR[:, 0:256], AF.Square, reads=[psRb], writes=[rsqb])
            d.update(r_sb=r_sb, r_sbb=r_sbb, rsq=rsq, rsqb=rsqb)

        def stage3(h):
            d = st.pop(h)
            r_sb, r_sbb, rsq, rsqb = d["r_sb"], d["r_sbb"], d["rsq"], d["rsqb"]
            psM, psMb = ctx["psring"].next()
            mm(S, nc, psM[:, 0:256], onesF, r_sb, True, True, [onesFb, r_sbb], [psMb], skip_group_check=True)
            mm(S, nc, psM[:, 256:512], onesF, rsq, True, True, [onesFb, rsqb], [psMb], skip_group_check=True)
            mean, meanb = f32r.next()
            var, varb = f32r.next()
            S.op("act", nc.scalar.mul, mean, psM[:, 0:256], 1.0 / 128.0, reads=[psMb], writes=[meanb])
            msq, msqb = rsq, rsqb
            S.op("dve", nc.vector.tensor_tensor, msq, mean, mean, ALU.mult, reads=[meanb], writes=[msqb])
            S.op("dve", nc.vector.scalar_tensor_tensor, var, psM[:, 256:512], 1.0 / 128.0, msq, ALU.mult, ALU.subtract,
                 reads=[psMb, msqb], writes=[varb])
            S.op("act", nc.scalar.activation, var, var, AF.Sqrt, bias=epsv, scale=1.0, reads=[varb, epsb], writes=[varb])
            S.op("dve", nc.vector.reciprocal, var, var, reads=[varb], writes=[varb])
            S.op("dve", nc.vector.tensor_tensor, r_sb, r_sb, mean, ALU.subtract, reads=[r_sbb, meanb], writes=[r_sbb])
            S.op("dve", nc.vector.tensor_tensor, r_sb, r_sb, var, ALU.mult, reads=[r_sbb, varb], writes=[r_sbb])
            S.op("dve", nc.vector.scalar_tensor_tensor, yR[:, h, i * 256:(i + 1) * 256], r_sb, gain[:, h:h + 1],
                 gs[:, h, i * 256:(i + 1) * 256], ALU.mult, ALU.mult, reads=[r_sbb, vecsb, gsb], writes=[yRb])

        for step in range(8 + 2):
            if step < 8:
                stage1(step)
            if 0 <= step - 1 < 8:
                stage2(step - 1)
            if 0 <= step - 2 < 8:
                stage3(step - 2)
        if i < 3:
            for hp in range(4):
                sl = slice(hp * 128, (hp + 1) * 128)
                S.op("dve", nc.vector.scalar_tensor_tensor, prev[:, sl], prev[:, sl], cdec[:, hp:hp + 1], kvs[:, i, sl],
                     ALU.mult, ALU.add, reads=[prevb, rscb, kvsb], writes=[prevb])
    S.dma("sp", osem, yR_d.rearrange("(h p) t -> p h t", p=128), yR, reads=[yRb])
    S.barrier()
    mem.release(base_mark)
    if stop == 1:
        return

    qT, qTb = mem.alloc("qT", (8, T), BF16)
    kTo, kTob = mem.alloc("kTo", (8, T), BF16)
    S.dma("sp", S.dma_sem("kTo"), kTo, P.din("kT_own", (1024, T), BF16).rearrange("(h p) t -> p h t", p=128), writes=[kTob])
    vo_d = P.din("v_own", (8 * T, 128), BF16)
    vo, vob = mem.alloc("vo", (NTT, 8, 128), BF16)
    vosem = S.dma_sem("vo")
    vo_v = vo_d.rearrange("(h tt p) d -> p tt h d", h=8, tt=NTT)
    for tt in range(NTT):
        S.dma("sp", vosem, vo[:, tt], vo_v[:, tt], writes=[vob])
    kmf, kmfb = load_const(ctx, "kmf", P.din("kmean_all", (128, 256)), (128, 8, 32), BF16, q="pool")
    gcon, gconb = load_const(ctx, "gcon", P.din("gconst", (128, 256)), (128, 2, 4, 32), F32)
    onehot_d = P.din("onehot", (32, 4096))
    oneh, onehb = load_const(ctx, "oneh", onehot_d, (32, 32, 128), BF16, q="pool", parts=32)
    tri, trib = load_const(ctx, "tri", P.din("tri", (128, 128)), (128, 128), BF16, q="pool")
    onesB, onesBb = mem.alloc("onesB", (128,), BF16)
    S.op("dve", nc.vector.memset, onesB, 1.0, writes=[onesBb])
    zerB, zerBb = mem.alloc("zerB", (512,), BF16)
    S.op("dve", nc.vector.memset, zerB, 0.0, writes=[zerBb])
    kT_all = P.din("kT_all", (1024, SEQ), BF16)
    v_all = P.din("v_all", (8 * SEQ, 128), BF16)
    wring = wslots_make(2, (NDC, 512), "wa")
    tokr = Ring([mem.alloc(f"tok{i}", (512,), BF16) for i in range(3)])
    tmp, _ = mem.alloc("rtmp", (4, 256), F32)
    tmpb = [Buf(f"rtmpa{i}") for i in range(4)]

    def evac_qa(g, tt, ps, pb):
        tk, tkb = tokr.next()
        rotary_evac(ctx, ps, pb, tk, tkb, 4, 128, 16, rope[:, tt, 0:16], rope[:, tt, 16:32], ropeb, tmp, tmpb)
        def post():
            pst, pstb = transpose_to(ctx, tk, tkb, 4, None, None, identb)
            S.op("act", nc.scalar.copy, qT[:, g * 4:(g + 1) * 4, tt * 128:(tt + 1) * 128],
                 pst.rearrange("p (h t) -> p h t", h=4), reads=[pstb], writes=[qTb])
        return post

    inproj_tokmajor(ctx, xT, xTb, wv, WL["q_a"], 512, 2, wring, evac_qa)
    att_mark = mem.mark()
    kbr = []
    vbr = []
    for i in range(2):
        t, b = mem.alloc(f"kb{i}", (4096,), BF16)
        kbr.append((t, b, S.dma_sem(f"kb{i}")))
        t, b = mem.alloc(f"vb{i}", (32, 128), BF16)
        vbr.append((t, b, S.dma_sem(f"vb{i}")))
    kbr, vbr = Ring(kbr), Ring(vbr)
    mskr = Ring([mem.alloc(f"msk{i}", (T,), BF16, parts=32) for i in range(2)])
    ptr = Ring([mem.alloc(f"pt{i}", (512,), BF16) for i in range(6)])
    gtmp = Ring([mem.alloc(f"gt{i}", (32,), F32) for i in range(4)])
    mx8, mx8b = mem.alloc("mx8", (8,), F32)
    mbb = Ring([mem.alloc(f"mb{i}", (32,), BF16) for i in range(2)])
    yAr = Ring([mem.alloc(f"yAh{i}", (T,), BF16) for i in range(2)])
    rsr = Ring([mem.alloc(f"rs{i}", (512,), F32) for i in range(2)])
    psO = PS[0:4]
    psS = Ring(PS[4:8])
    gate_state = {}

    def gate_pre(hh, tt, msk_t):
        j = tt // 2
        psg, psgb = psS.next()
        mm(S, nc, psg[:, 0:32], qT[:, hh, tt * 128:(tt + 1) * 128], kmf[:, hh, :], True, True, [qTb, kmfb], [psgb])
        gm, gmb = gtmp.next()
        S.op("dve", nc.vector.tensor_tensor, gm, psg[:, 0:32], gcon[:, 0, j, :], ALU.add,
             reads=[psgb, gconb], writes=[gmb])
        S.op("dve", nc.vector.max, mx8, gm, reads=[gmb], writes=[mx8b])
        sel, selb = gtmp.next()
        S.op("dve", nc.vector.tensor_single_scalar, sel, gm, mx8[:, 2:3], ALU.is_ge,
             reads=[gmb, mx8b], writes=[selb])
        S.op("dve", nc.vector.tensor_tensor, sel, sel, gcon[:, 1, j, :], ALU.mult,
             reads=[selb, gconb], writes=[selb])
        mb, mbB = mbb.next()
        S.op("dve", nc.vector.tensor_scalar, mb, sel, 1.0, MASKV, ALU.subtract, ALU.mult,
             reads=[selb], writes=[mbB])
        gate_state[(hh, tt)] = (mb, mbB)

    def gate_post(hh, tt, msk_t):
        msk_, mskb_ = msk_t
        mb, mbB = gate_state.pop((hh, tt))
        pst_, pstb_ = psS.next()
        pstv = pst_[:, 0:64].bitcast(BF16)
        S.op("pe", nc.tensor.transpose, pstv[0:32, 0:128], mb, ident, reads=[mbB, identB], writes=[pstb_], pe_accum=True)
        S.op("act", nc.scalar.copy, msk_[0:32, tt * 128:(tt + 1) * 128], pstv[0:32, 0:128], reads=[pstb_], writes=[mskb_])

    msk_next = mskr.next()
    for tt in range(NTT):
        gate_pre(0, tt, msk_next)
        gate_post(0, tt, msk_next)
    for h in range(8):
        msk, mskb = msk_next
        if h < 7:
            msk_next = mskr.next()
        for j in range(4):
            mm(S, nc, psO[j][0], zerB[:, 0:128], zerB, True, False, [zerBb], [psO[j][1]], skip_group_check=True)
        items = []
        for half in range(2):
            kb, kbB, kbsem = kbr.next()
            vb, vbB, vbsem = vbr.next()
            S.dma("sp", kbsem, kb, kT_all[h * 128:(h + 1) * 128, half * 4096:(half + 1) * 4096], writes=[kbB])
            S.dma("act", vbsem, vb,
                  v_all[h * SEQ + half * 4096: h * SEQ + (half + 1) * 4096, :].rearrange("(kc p) d -> p kc d", p=128),
                  writes=[vbB])
            for g2 in range(2):
                for kc in range(32):
                    blk = (half * 32 + kc) // 2
                    items.append(dict(kind="g", g2=g2, nq=512, q0=g2 * 512, o0=0,
                                      lhs=kb[:, kc * 128:(kc + 1) * 128], lhsb=kbB, blk=blk,
                                      v=vb[:, kc, :], vbuf=vbB, last=False))
        for j in range(4):
            items.append(dict(kind="o", g2=j // 2, nq=256, q0=j * 256, o0=(j % 2) * 256,
                              lhs=kTo[:, h, j * 256: j * 256 + 128], lhsb=kTob,
                              v=vo[:, 2 * j, h, :], vbuf=vob, last=False))
            items.append(dict(kind="o", g2=j // 2, nq=128, q0=j * 256 + 128, o0=(j % 2) * 256 + 128,
                              lhs=kTo[:, h, j * 256 + 128:(j + 1) * 256], lhsb=kTob,
                              v=vo[:, 2 * j + 1, h, :], vbuf=vob, last=True))

        def emit_qk(it):
            pss, pssb = psS.next()
            nq = it["nq"]
            mm(S, nc, pss[:, 0:nq], it["lhs"], qT[:, h, it["q0"]:it["q0"] + nq], True, False, [it["lhsb"], qTb], [pssb])
            if it["kind"] == "g":
                mm(S, nc, pss[:, 0:nq], oneh[0:32, it["blk"], :], msk[0:32, it["q0"]:it["q0"] + nq], False, True,
                   [onehb, mskb], [pssb])
            else:
                mm(S, nc, pss[:, 0:128], ident, tri, False, True, [identB, trib], [pssb], skip_group_check=True)
            pt, ptb = ptr.next()
            S.op("act", nc.scalar.activation, pt[:, 0:nq], pss[:, 0:nq], AF.Exp, scale=ATT_SCALE,
                 reads=[pssb], writes=[ptb])
            it["pt"], it["ptb"] = pt, ptb

        def emit_pv(it):
            g2, nq, o0 = it["g2"], it["nq"], it["o0"]
            pt, ptb = it["pt"], it["ptb"]
            mm(S, nc, psO[2 * g2][0][:, o0:o0 + nq], it["v"], pt[:, 0:nq], False, it["last"], [it["vbuf"], ptb],
               [psO[2 * g2][1]], skip_group_check=True)
            mm(S, nc, psO[2 * g2 + 1][0][:, o0:o0 + nq], onesB, pt[:, 0:nq], False, it["last"], [onesBb, ptb],
               [psO[2 * g2 + 1][1]], skip_group_check=True)

        PDEPTH = 3
        for idx, it in enumerate(items):
            emit_qk(it)
            if idx >= PDEPTH:
                emit_pv(items[idx - PDEPTH])
            if h < 7 and idx >= 8 and (idx - 8) % 16 == 0 and (idx - 8) // 16 < NTT:
                gate_pre(h + 1, (idx - 8) // 16, msk_next)
            if h < 7 and idx >= 16 and (idx - 16) % 16 == 0 and (idx - 16) // 16 < NTT:
                gate_post(h + 1, (idx - 16) // 16, msk_next)
        for it in items[len(items) - PDEPTH:]:
            emit_pv(it)
        yAh, yAhb = yAr.next()
        for g2 in range(2):
            rs, rsb = rsr.next()
            S.op("dve", nc.vector.reciprocal, rs, psO[2 * g2 + 1][0], reads=[psO[2 * g2 + 1][1]], writes=[rsb])
            S.op("dve", nc.vector.tensor_tensor, yAh[:, g2 * 512:(g2 + 1) * 512], psO[2 * g2][0], rs, ALU.mult,
                 reads=[psO[2 * g2][1], rsb], writes=[yAhb])
        S.dma("sp", osem, yA_d[h * 128:(h + 1) * 128, :], yAh, reads=[yAhb])
    S.barrier()
    mem.release(base_mark)
    if stop == 2:
        return
    prog_L2(ctx, locals())


def layer_norm_fm(ctx, acc, accb, gcol, bcol, vecsb, onesF, onesFb, epsv, epsb, hT=None, hTb=None):
    S, nc, mem = ctx["S"], ctx["nc"], ctx["mem"]
    mk = mem.mark()
    sqr = Ring([mem.alloc(f"lnsq{i}", (256,), F32) for i in range(2)])
    mean, meanb = mem.alloc("lnmean", (256,), F32)
    rstd, rstdb = mem.alloc("lnrstd", (256,), F32)
    msq, msqb = mem.alloc("lnmsq", (256,), F32)
    psr = ctx["psring"]
    for th in range(4):
        ts = slice(th * 256, (th + 1) * 256)
        psM, psMb = psr.next()
        psV, psVb = psr.next()
        for oc in range(NDC):
            mm(S, nc, psM[:, 0:256], onesF, acc[:, oc, ts], oc == 0, oc == NDC - 1, [onesFb, accb], [psMb])
        for oc in range(NDC):
            sq, sqb = sqr.next()
            S.op("act", nc.scalar.activation, sq, acc[:, oc, ts], AF.Square, reads=[accb], writes=[sqb])
            mm(S, nc, psV[:, 0:256], onesF, sq, oc == 0, oc == NDC - 1, [onesFb, sqb], [psVb])
        S.op("act", nc.scalar.mul, mean, psM[:, 0:256], 1.0 / D, reads=[psMb], writes=[meanb])
        S.op("dve", nc.vector.tensor_tensor, msq, mean, mean, ALU.mult, reads=[meanb], writes=[msqb])
        S.op("dve", nc.vector.scalar_tensor_tensor, rstd, psV[:, 0:256], 1.0 / D, msq, ALU.mult, ALU.subtract,
             reads=[psVb, msqb], writes=[rstdb])
        S.op("act", nc.scalar.activation, rstd, rstd, AF.Sqrt, bias=epsv, scale=1.0, reads=[rstdb, epsb], writes=[rstdb])
        S.op("dve", nc.vector.reciprocal, rstd, rstd, reads=[rstdb], writes=[rstdb])
        for oc in range(NDC):
            a = acc[:, oc, ts]
            S.op("dve", nc.vector.tensor_tensor, a, a, mean, ALU.subtract, reads=[accb, meanb], writes=[accb])
            S.op("dve", nc.vector.tensor_tensor, a, a, rstd, ALU.mult, reads=[accb, rstdb], writes=[accb])
            S.op("dve", nc.vector.tensor_scalar, a, a, gcol[:, oc:oc + 1], bcol[:, oc:oc + 1], ALU.mult, ALU.add,
                 reads=[accb, vecsb], writes=[accb])
            if hT is not None:
                S.op("act", nc.scalar.copy, hT[:, oc, ts], a, reads=[accb], writes=[hTb])
    mem.release(mk)


def prog_L2(ctx, L):
    P, nc, S, mem, PS = ctx["P"], ctx["nc"], ctx["S"], ctx["mem"], ctx["PS"]
    stop = ctx.get("stop")
    xT, xTb, wv, vecs, vecsb = L["xT"], L["xTb"], L["wv"], L["vecs"], L["vecsb"]
    yR_d, yA_d, yP_d, xo_d, osem, xT_d = L["yR_d"], L["yA_d"], L["yP_d"], L["xo_d"], L["osem"], L["xT_d"]
    base_mark = L["base_mark"]
    onesF, onesFb, epsv, epsb = L["onesF"], L["onesFb"], L["epsv"], L["epsb"]
    pscale = L["pscale"]
    ctx["psring"] = Ring(PS)
    psr = ctx["psring"]

    def wslots_make(n, shape, nm):
        sl = []
        for i in range(n):
            t, b = mem.alloc(f"{nm}{i}", shape, BF16)
            sl.append((t, b, S.dma_sem(f"{nm}{i}")))
        return Ring(sl)

    pext, pextb = mem.alloc("pext", (8, 16 + T), F32)
    S.dma("sp", S.dma_sem("halo"), pext[:, :, 0:16], P.din("halo_in", (128, 128)).rearrange("p (a b) -> p a b", a=8),
          writes=[pextb])
    invn, invnb = load_const(ctx, "invn", P.din("invn", (128, 64)), (128, 4, 16), F32)
    wp, wpb = mem.alloc("wp", (4, 2, 256), BF16)
    S.dma("pool", S.dma_sem("wp"), wp, P.din("w_pool", (1024, 256)).rearrange("(g cc p) e -> p g cc e", g=4, cc=2),
          writes=[wpb])
    wring = wslots_make(2, (NDC, 512), "wpi")
    for g in range(2):
        wt, wb, wsem = wring.next()
        S.dma("pool", wsem, wt, wv[:, :, WL["p_in"] + g * 512: WL["p_in"] + (g + 1) * 512], writes=[wb])
        for cc in range(4):
            for th in range(2):
                ps, pb = psr.next()
                for dc in range(NDC):
                    mm(S, nc, ps, wt[:, dc, cc * 128:(cc + 1) * 128], xT[:, dc, th * 512:(th + 1) * 512],
                       dc == 0, dc == NDC - 1, [wb, xTb], [pb])
                S.op("act", nc.scalar.copy, pext[:, g * 4 + cc, 16 + th * 512: 16 + (th + 1) * 512], ps,
                     reads=[pb], writes=[pextb])
    sA, sAb = mem.alloc("sA", (2, 16 + T), F32)
    sB, sBb = mem.alloc("sB", (2, 16 + T), F32)
    dT, dTb = mem.alloc("dT", (2, T), BF16)
    yP, yPb = mem.alloc("yP", (8, T), BF16)
    NX = 16 + T
    for gi, w in enumerate((2, 4, 8, 16)):
        src, srcb = pext[:, 2 * gi:2 * gi + 2, :], pextb
        k = 1
        bufs = [(sA, sAb), (sB, sBb)]
        bi = 0
        while k < w:
            dst, dstb = bufs[bi]
            S.op("dve", nc.vector.tensor_tensor, dst[:, :, k:NX], src[:, :, k:NX], src[:, :, 0:NX - k], ALU.add,
                 reads=[srcb], writes=[dstb])
            if k > 1 or True:
                pass
            src, srcb = dst, dstb
            bi ^= 1
            k *= 2
        S.op("dve", nc.vector.scalar_tensor_tensor, dT, src[:, :, 16:NX], 1.0 / w, pext[:, 2 * gi:2 * gi + 2, 16:NX],
             ALU.mult, ALU.subtract, reads=[srcb, pextb], writes=[dTb])
        tmp16, tmp16b = bufs[bi]
        S.op("dve", nc.vector.tensor_tensor, tmp16[:, :, 0:16], src[:, :, 16:32],
             invn[:, gi, :].unsqueeze(1).broadcast_to([128, 2, 16]), ALU.mult, reads=[srcb, invnb], writes=[tmp16b])
        S.op("dve", nc.vector.tensor_tensor, dT[:, :, 0:16], tmp16[:, :, 0:16], pext[:, 2 * gi:2 * gi + 2, 16:32],
             ALU.subtract, reads=[tmp16b, pextb], writes=[dTb])
        for ec in range(2):
            for th in range(2):
                ps, pb = psr.next()
                for cc in range(2):
                    mm(S, nc, ps, wp[:, gi, cc, ec * 128:(ec + 1) * 128], dT[:, cc, th * 512:(th + 1) * 512],
                       cc == 0, cc == 1, [wpb, dTb], [pb])
                ch = 2 * gi + ec
                S.op("act", nc.scalar.activation, yP[:, ch, th * 512:(th + 1) * 512], ps, AF.Copy,
                     scale=pscale[:, ch:ch + 1], reads=[pb, vecsb], writes=[yPb])
    S.dma("sp", osem, yP_d.rearrange("(h p) t -> p h t", p=128), yP, reads=[yPb])
    S.barrier()
    mem.release(base_mark)
    if stop == 3:
        return

    mem.peak = mem.top
    merged, mergedb = mem.alloc_top("merged", (NDC, T), BF16)
    br, brb = mem.alloc("br", (3, 8, T), BF16)
    bsem = S.dma_sem("br")
    for n, d_ in enumerate((yA_d, yR_d, yP_d)):
        S.dma("sp", bsem, br[:, n], d_.rearrange("(h p) t -> p h t", p=128), writes=[brb])
    w_br = P.din("w_branch", (3 * 1024, D))
    wbv = w_br.rearrange("(n kc p) c -> p n kc c", n=3, kc=8)
    wgr = wslots_make(2, (3, NDC, 128), "wg")
    wbr = wslots_make(2, (3, 8, 128), "wb")
    sgr = Ring([mem.alloc(f"sg{i}", (512,), F32) for i in range(3)])
    mtr = Ring([mem.alloc(f"mt{i}", (512,), F32) for i in range(3)])
    for dch in range(NDC):
        wg, wgb, wgsem = wgr.next()
        wb_, wbb, wbsem = wbr.next()
        for n in range(3):
            c0 = WL["gate"] + n * D + dch * 128
            S.dma("pool", wgsem, wg[:, n], wv[:, :, c0:c0 + 128], writes=[wgb])
            S.dma("pool", wbsem, wb_[:, n], wbv[:, n, :, dch * 128:(dch + 1) * 128], writes=[wbb])
        for th in range(2):
            ts = slice(th * 512, (th + 1) * 512)
            prods = []
            for n in range(3):
                psG, psGb = psr.next()
                for dc in range(NDC):
                    mm(S, nc, psG, wg[:, n, dc, :], xT[:, dc, ts], dc == 0, dc == NDC - 1, [wgb, xTb], [psGb])
                psB_, psBb = psr.next()
                for kc in range(8):
                    mm(S, nc, psB_, wb_[:, n, kc, :], br[:, n, kc, ts], kc == 0, kc == 7, [wbb, brb], [psBb])
                sg, sgb = sgr.next()
                S.op("act", nc.scalar.activation, sg, psG, AF.Sigmoid, reads=[psGb], writes=[sgb])
                S.op("dve", nc.vector.tensor_tensor, sg, sg, psB_, ALU.mult, reads=[sgb, psBb], writes=[sgb])
                prods.append((sg, sgb))
            mt, mtb = mtr.next()
            S.op("dve", nc.vector.tensor_tensor, mt, prods[0][0], prods[1][0], ALU.add,
                 reads=[prods[0][1], prods[1][1]], writes=[mtb])
            S.op("dve", nc.vector.tensor_tensor, merged[:, dch, ts], mt, prods[2][0], ALU.add,
                 reads=[mtb, prods[2][1]], writes=[mergedb])
    if stop == 4:
        S.dma("sp", osem, xo_d.rearrange("(c p) t -> p c t", p=128)[:, :, 0:512],
              merged.bitcast(F32), reads=[mergedb])
        return
    S.barrier()
    assert mem.peak <= mem.top_limit, (mem.peak, mem.top_limit)
    mem.release(base_mark)

    acc, accb = mem.alloc("acc", (NDC, T), F32)
    hT, hTb = xT, xTb
    mk_o = mem.mark()
    w_out = P.din("w_out", (D, D))
    wov = w_out.rearrange("(kc p) c -> p kc c", p=128)
    wor = wslots_make(2, (NDC, 128), "wo")
    xrr = []
    for i in range(3):
        t, b = mem.alloc(f"xr{i}", (512,), F32)
        xrr.append((t, b, S.dma_sem(f"xr{i}")))
    xrr = Ring(xrr)
    xTv = xT_d.rearrange("(c p) t -> p c t", p=128)
    for oc in range(NDC):
        wo, wob, wosem = wor.next()
        S.dma("pool", wosem, wo, wov[:, :, oc * 128:(oc + 1) * 128], writes=[wob])
        for th in range(2):
            ts = slice(th * 512, (th + 1) * 512)
            xr, xrb, xrsem = xrr.next()
            S.dma("sp", xrsem, xr, xTv[:, oc, ts], writes=[xrb])
            ps, pb = psr.next()
            for kc in range(NDC):
                mm(S, nc, ps, wo[:, kc, :], merged[:, kc, ts], kc == 0, kc == NDC - 1, [wob, mergedb], [pb])
            S.op("dve", nc.vector.scalar_tensor_tensor, acc[:, oc, ts], xr, ALPHA, ps, ALU.mult, ALU.add,
                 reads=[xrb, pb], writes=[accb])
    mem.release(mk_o)
    if stop == 45:
        S.dma("sp", osem, xo_d.rearrange("(c p) t -> p c t", p=128), acc, reads=[accb])
        return
    layer_norm_fm(ctx, acc, accb, L["ln1g"], L["ln1b"], vecsb, onesF, onesFb, epsv, epsb, hT, hTb)
    if stop == 5:
        S.dma("sp", osem, xo_d.rearrange("(c p) t -> p c t", p=128), acc, reads=[accb])
        return
    S.barrier()
    prog_MoE(ctx, L, acc, accb, hT, hTb)
    with_A = ctx["P"].mode == "LA"
    layer_norm_fm(ctx, acc, accb, L["ln2g"], L["ln2b"], vecsb, onesF, onesFb, epsv, epsb,
                  hT if with_A else None, hTb if with_A else None)
    S.dma("sp", osem, xo_d.rearrange("(c p) t -> p c t", p=128), acc, reads=[accb])
    if with_A:
        S.barrier()
        mem.release(base_mark)
        ctx["psring"] = Ring(PS)
        A_body(ctx, L["rsc"], L["rscb"], "o_")


def prog_MoE(ctx, L, acc, accb, hT, hTb):
    P, nc, S, mem, PS = ctx["P"], ctx["nc"], ctx["S"], ctx["mem"], ctx["PS"]
    psr = ctx["psring"]
    osem = L["osem"]
    mk = mem.mark()
    wr, wrb = mem.alloc("wr", (NDC, 36), F32)
    S.dma("sp", S.dma_sem("wr"), wr, P.din("w_r", (D, 36)).rearrange("(dc p) n -> p dc n", p=128), writes=[wrb])
    brr, brrb = load_const(ctx, "brr", P.din("b_r", (128, 36)), (128, 36), F32)
    oneh, onehb = mem.alloc("oneh2", (32, 128), BF16, parts=32)
    S.dma("pool", S.dma_sem("oneh2"), oneh, L["onehot_d"].rearrange("p (a b) -> p a b", a=32), writes=[onehb])
    ident, identB = ctx["C"]["ident"]
    whi, whib = mem.alloc("whi", (T,), BF16, parts=32)
    wlo, wlob = mem.alloc("wlo", (T,), BF16, parts=32)
    rt = {}
    for nm, n in [("lg", 36), ("m1", 1), ("nm1", 1), ("e1", 4), ("s1", 1), ("gtop", 1), ("gmask", 4), ("gpen", 4),
                  ("l2m", 32), ("mx8", 8), ("nmx", 1), ("e2", 1), ("den", 1), ("wA", 1), ("wB", 1), ("t1", 32),
                  ("t2", 32), ("wf", 32), ("hif", 32)]:
        rt[nm] = mem.alloc("r_" + nm, (n,), F32)
    hi_, hib = mem.alloc("r_hi", (32,), BF16)
    lo_, lob = mem.alloc("r_lo", (32,), BF16)

    def V(op, out, *args, **kw):
        return S.op("dve", op, out[0], *args, writes=[out[1]], **kw)

    for tt in range(NTT):
        psR, psRb = psr.next()
        for dc in range(NDC):
            mm(S, nc, psR[:, 0:36], acc[:, dc, tt * 128:(tt + 1) * 128], wr[:, dc, :], dc == 0, dc == NDC - 1,
               [accb, wrb], [psRb])
        lg, lgb = rt["lg"]
        V(nc.vector.tensor_tensor, rt["lg"], psR[:, 0:36], brr, ALU.add, reads=[psRb, brrb])
        V(nc.vector.tensor_reduce, rt["m1"], lg[:, 0:4], AX.X, ALU.max, reads=[lgb])
        V(nc.vector.tensor_single_scalar, rt["nm1"], rt["m1"][0], -1.0, ALU.mult, reads=[rt["m1"][1]])
        S.op("act", nc.scalar.activation, rt["e1"][0], lg[:, 0:4], AF.Exp, bias=rt["nm1"][0], scale=1.0,
             accum_out=rt["s1"][0], reads=[lgb, rt["nm1"][1]], writes=[rt["e1"][1], rt["s1"][1]])
        V(nc.vector.reciprocal, rt["gtop"], rt["s1"][0], reads=[rt["s1"][1]])
        V(nc.vector.tensor_single_scalar, rt["gmask"], lg[:, 0:4], rt["m1"][0], ALU.is_ge, reads=[lgb, rt["m1"][1]])
        V(nc.vector.tensor_scalar, rt["gpen"], rt["gmask"][0], 1.0, 1e30, ALU.subtract, ALU.mult, reads=[rt["gmask"][1]])
        l2m, l2mb = rt["l2m"]
        S.op("dve", nc.vector.tensor_tensor, l2m.rearrange("p (g j) -> p g j", g=4),
             lg[:, 4:36].rearrange("p (g j) -> p g j", g=4),
             rt["gpen"][0].unsqueeze(2).broadcast_to([128, 4, 8]), ALU.add,
             reads=[lgb, rt["gpen"][1]], writes=[l2mb])
        mx8, mx8b = rt["mx8"]
        V(nc.vector.max, rt["mx8"], l2m, reads=[l2mb])
        V(nc.vector.tensor_single_scalar, rt["nmx"], mx8[:, 0:1], -1.0, ALU.mult, reads=[mx8b])
        S.op("act", nc.scalar.activation, rt["e2"][0], mx8[:, 1:2], AF.Exp, bias=rt["nmx"][0], scale=1.0,
             reads=[mx8b, rt["nmx"][1]], writes=[rt["e2"][1]])
        V(nc.vector.tensor_single_scalar, rt["den"], rt["e2"][0], 1.0, ALU.add, reads=[rt["e2"][1]])
        V(nc.vector.reciprocal, rt["den"], rt["den"][0], reads=[rt["den"][1]])
        V(nc.vector.tensor_tensor, rt["wA"], rt["gtop"][0], rt["den"][0], ALU.mult, reads=[rt["gtop"][1], rt["den"][1]])
        V(nc.vector.tensor_tensor, rt["wB"], rt["wA"][0], rt["e2"][0], ALU.mult, reads=[rt["wA"][1], rt["e2"][1]])
        V(nc.vector.tensor_scalar, rt["t1"], l2m, mx8[:, 0:1], rt["wA"][0], ALU.is_equal, ALU.mult,
          reads=[l2mb, mx8b, rt["wA"][1]])
        V(nc.vector.tensor_scalar, rt["t2"], l2m, mx8[:, 1:2], rt["wB"][0], ALU.is_equal, ALU.mult,
          reads=[l2mb, mx8b, rt["wB"][1]])
        V(nc.vector.tensor_tensor, rt["wf"], rt["t1"][0], rt["t2"][0], ALU.add, reads=[rt["t1"][1], rt["t2"][1]])
        S.op("act", nc.scalar.copy, hi_, rt["wf"][0], reads=[rt["wf"][1]], writes=[hib])
        S.op("dve", nc.vector.tensor_tensor, lo_, rt["wf"][0], hi_, ALU.subtract, reads=[rt["wf"][1], hib], writes=[lob])
        for src, srcb, dst, dstb in ((hi_, hib, whi, whib), (lo_, lob, wlo, wlob)):
            pst_, pstb_ = psr.next()
            pstv = pst_[:, 0:64].bitcast(BF16)
            S.op("pe", nc.tensor.transpose, pstv[0:32, 0:128], src, ident, reads=[srcb, identB], writes=[pstb_], pe_accum=True)
            S.op("act", nc.scalar.copy, dst[0:32, tt * 128:(tt + 1) * 128], pstv[0:32, 0:128], reads=[pstb_], writes=[dstb])
    for oc in range(NDC):
        S.op("act", nc.scalar.mul, acc[:, oc, :], acc[:, oc, :], ALPHA, reads=[accb], writes=[accb])
    if ctx.get("stop") == 6:
        return

    def wslots(n, shape, nm):
        sl = []
        for i in range(n):
            t, b = mem.alloc(f"{nm}{i}", shape, BF16)
            sl.append((t, b, S.dma_sem(f"{nm}{i}")))
        return Ring(sl)

    wgur = wslots(4, (NDC, 256), "wgu")
    wdr = wslots(2, (4, 1024), "wd")
    aT, aTb = mem.alloc("aT", (4, T), BF16)
    wbcr = Ring([mem.alloc(f"wbc{i}", (T,), F32) for i in range(2)])
    sgr = Ring([mem.alloc(f"msg{i}", (512,), F32) for i in range(2)])
    uwr = Ring([mem.alloc(f"muw{i}", (512,), F32) for i in range(2)])
    w_eg = P.din("w_eg", (32 * D, 512)).rearrange("(e dc p) f -> e p dc f", e=32, p=128)
    w_eu = P.din("w_eu", (32 * D, 512)).rearrange("(e dc p) f -> e p dc f", e=32, p=128)
    w_ed = P.din("w_ed", (32 * 512, D)).rearrange("(e fc p) o -> e p fc o", e=32, p=128)
    n_exp = int(ctx.get("n_exp", 32))
    for e in range(n_exp):
        wbc, wbcb = wbcr.next()
        for th in range(2):
            ts = slice(th * 512, (th + 1) * 512)
            ps, pb = psr.next()
            mm(S, nc, ps, oneh[0:32, e, :], whi[0:32, ts], True, False, [onehb, whib], [pb])
            mm(S, nc, ps, oneh[0:32, e, :], wlo[0:32, ts], False, True, [onehb, wlob], [pb])
            S.op("act", nc.scalar.copy, wbc[:, ts], ps, reads=[pb], writes=[wbcb])
        for fh in range(2):
            wg, wgb, wgsem = wgur.next()
            wu, wub, wusem = wgur.next()
            S.dma("pool", wgsem, wg, w_eg[e][:, :, fh * 256:(fh + 1) * 256], writes=[wgb])
            S.dma("pool", wusem, wu, w_eu[e][:, :, fh * 256:(fh + 1) * 256], writes=[wub])
            for fq in range(2):
                fc = fh * 2 + fq
                for th in range(2):
                    ts = slice(th * 512, (th + 1) * 512)
                    psG, psGb = psr.next()
                    for dc in range(NDC):
                        mm(S, nc, psG, wg[:, dc, fq * 128:(fq + 1) * 128], hT[:, dc, ts], dc == 0, dc == NDC - 1,
                           [wgb, hTb], [psGb])
                    psU, psUb = psr.next()
                    for dc in range(NDC):
                        mm(S, nc, psU, wu[:, dc, fq * 128:(fq + 1) * 128], hT[:, dc, ts], dc == 0, dc == NDC - 1,
                           [wub, hTb], [psUb])
                    sg, sgb = sgr.next()
                    uw, uwb = uwr.next()
                    S.op("act", nc.scalar.activation, sg, psG, AF.Silu, reads=[psGb], writes=[sgb])
                    S.op("dve", nc.vector.tensor_tensor, uw, psU, wbc[:, ts], ALU.mult, reads=[psUb, wbcb], writes=[uwb])
                    S.op("dve", nc.vector.tensor_tensor, aT[:, fc, ts], sg, uw, ALU.mult, reads=[sgb, uwb], writes=[aTb])
        for oh in range(2):
            wd, wdb, wdsem = wdr.next()
            S.dma("pool", wdsem, wd, w_ed[e][:, :, oh * 1024:(oh + 1) * 1024], writes=[wdb])
            for oq in range(8):
                oc = oh * 8 + oq
                for th in range(2):
                    ts = slice(th * 512, (th + 1) * 512)
                    psY, psYb = psr.next()
                    for fc in range(4):
                        mm(S, nc, psY, wd[:, fc, oq * 128:(oq + 1) * 128], aT[:, fc, ts], fc == 0, fc == 3,
                           [wdb, aTb], [psYb])
                    S.op("dve", nc.vector.tensor_tensor, acc[:, oc, ts], acc[:, oc, ts], psY, ALU.add,
                         reads=[accb, psYb], writes=[accb])
    S.barrier()
    mem.release(mk)


def layer_inputs(inp, l):
    w_in = inp["w_in"][l]
    m = {}
    m["w_l"] = wl_from_w_in(w_in)
    m["w_a"] = np.ascontiguousarray(np.concatenate([w_in[:, 1024:3072], w_in[:, 3584:5120], w_in[:, 6144:7168]], axis=1))
    m["vecs"] = np.ascontiguousarray(np.concatenate(
        [feat_vec(inp["ret_gain"][l]), feat_vec(inp["pool_scale"][l]), feat_vec(inp["ln1_g"][l]), feat_vec(inp["ln1_b"][l]),
         feat_vec(inp["ln2_g"][l]), feat_vec(inp["ln2_b"][l])], axis=1).astype(np.float32))
    m["w_pool"] = np.ascontiguousarray(inp["w_pool"][l].reshape(1024, 256))
    m["w_branch"] = np.ascontiguousarray(inp["w_branch"][l].reshape(3072, D))
    m["w_out"] = np.ascontiguousarray(inp["w_out"][l])
    m["w_r"] = np.ascontiguousarray(np.concatenate([inp["w_r1"][l], inp["w_r2"][l]], axis=1))
    m["b_r"] = np.ascontiguousarray(np.broadcast_to(np.concatenate([inp["b_r1"][l], inp["b_r2"][l]])[None, :], (128, 36)))
    m["w_eg"] = np.ascontiguousarray(inp["w_e_gate"][l].reshape(32 * D, 512))
    m["w_eu"] = np.ascontiguousarray(inp["w_e_up"][l].reshape(32 * D, 512))
    m["w_ed"] = np.ascontiguousarray(inp["w_e_down"][l].reshape(32 * 512, D))
    return m


def gather_payload(outs):
    kT_all = np.concatenate([o["kT_loc"] for o in outs], axis=1)
    v_all = np.concatenate([o["v_loc"].reshape(8, T, 128) for o in outs], axis=1).reshape(8 * SEQ, 128)
    kmean_all = np.concatenate([o["kmean"].reshape(128, 8, 4) for o in outs], axis=2).reshape(128, 256)
    S_all = np.concatenate([o["S_loc"] for o in outs], axis=0)
    res = []
    for c in range(NCORES):
        o = outs[c]
        res.append(dict(kT_all=kT_all, v_all=v_all, kmean_all=kmean_all, S_all=S_all,
                        kT_own=o["kT_loc"], v_own=o["v_loc"], krT2=o["krT2"], vr_tok=o["vr_tok"], kv_loc=o["kv_loc"],
                        halo_in=(outs[c - 1]["halo"] if c > 0 else np.zeros((128, 128), np.float32))))
    return res


_PROGS = {}


def _prog(mode):
    if mode not in _PROGS:
        _PROGS[mode] = build(mode)
    return _PROGS[mode]


def kernel(x, w_in, ret_gain, w_pool, pool_scale, w_branch, w_out, ln1_g, ln1_b,
           w_r1, b_r1, w_r2, b_r2, w_e_gate, w_e_up, w_e_down, ln2_g, ln2_b):
    inp = dict(w_in=w_in, ret_gain=ret_gain, w_pool=w_pool, pool_scale=pool_scale, w_branch=w_branch, w_out=w_out,
               ln1_g=ln1_g, ln1_b=ln1_b, w_r1=w_r1, b_r1=b_r1, w_r2=w_r2, b_r2=b_r2,
               w_e_gate=w_e_gate, w_e_up=w_e_up, w_e_down=w_e_down, ln2_g=ln2_g, ln2_b=ln2_b)
    inp = {k: np.asarray(v, dtype=np.float32) for k, v in inp.items()}
    x = np.asarray(x, dtype=np.float32)
    PA, PLA, PL = _prog("A"), _prog("LA"), _prog("L")
    cores = list(range(NCORES))
    tabs = [host_tables(c) for c in cores]
    xT = [np.ascontiguousarray(x[0, c * T:(c + 1) * T].T) for c in cores]
    A_OUT = ["kT_loc", "v_loc", "kmean", "krT2", "vr_tok", "kv_loc", "S_loc", "halo"]
    li = layer_inputs(inp, 0)
    maps = []
    for c in cores:
        m = {**li, **tabs[c], "xT": xT[c]}
        maps.append({k: m[k] for k in PA.ins})
    ra = run_bass_kernel_spmd(PA.nc, maps, core_ids=cores).results
    for l in range(DEPTH):
        pay = gather_payload(ra)
        last = l == DEPTH - 1
        PX = PL if last else PLA
        nxt = None if last else layer_inputs(inp, l + 1)
        maps = []
        for c in cores:
            m = {**li, **tabs[c], **pay[c], "xT": xT[c]}
            if not last:
                m["w_a"] = nxt["w_a"]
            maps.append({k: m[k] for k in PX.ins})
        rl = run_bass_kernel_spmd(PX.nc, maps, core_ids=cores).results
        xT = [np.asarray(rl[c]["xT_out"], dtype=np.float32) for c in cores]
        if not last:
            ra = [{k: rl[c]["o_" + k] for k in A_OUT} for c in cores]
        li = nxt
        del maps, pay, rl
    out = np.concatenate([xT[c].T for c in cores], axis=0)[None]
    return np.ascontiguousarray(out.astype(np.float32))
```

```python
import contextlib
import math
import numpy as np
import ml_dtypes
import concourse.bass as bass
import concourse.mybir as mybir
from concourse.bass_utils import run_bass_kernel_spmd
from concourse.alu_op_type import AluOpType as ALU

AF = mybir.ActivationFunctionType
AX = mybir.AxisListType
F32 = mybir.dt.float32
BF16 = mybir.dt.bfloat16
NPBF = ml_dtypes.bfloat16

NCORES = 8
D = 2048
SEQ = 8192
T = 1024
NTT = 8
NDC = 16
DEPTH = 4
N_IN = 13312
COL = dict(q_a=0, k_a=1024, v_a=2048, q_r=3072, k_r=3584, v_r=4096, g_r=5120, p_in=6144, gate=7168)
ALPHA = (2 * DEPTH) ** 0.25
LN_EPS = 1e-5
MASKV = 30000.0
ATT_SCALE = 128 ** -0.5
NEGBIG = -1e30


class Buf:
    __slots__ = ("name", "w", "r", "excl")

    def __init__(self, name, excl=False):
        self.name = name
        self.w = None
        self.r = []
        self.excl = excl


class Sched:
    def __init__(self, nc, stack):
        self.nc = nc
        self.stack = stack
        self.engs = {"pe": nc.tensor, "act": nc.scalar, "dve": nc.vector,
                     "pool": nc.gpsimd, "sp": nc.sync}
        self.sems = {}
        self.cnt = {}
        for k in self.engs:
            self.sems[k] = stack.enter_context(nc.semaphore("s_" + k))
            self.cnt[k] = 0
        self.waited = {k: {} for k in self.engs}
        self.n_wait = 0
        self.n_ins = 0

    def dma_sem(self, name):
        key = "dma_" + name
        self.sems[key] = self.stack.enter_context(self.nc.semaphore(key))
        self.cnt[key] = 0
        return key

    def _wait(self, eng, ev):
        if ev is None:
            return
        key, val = ev[0], ev[1]
        if self.waited[eng].get(key, 0) >= val:
            return
        self.engs[eng].wait_ge(self.sems[key], val)
        self.waited[eng][key] = val
        self.n_wait += 1

    def _deps(self, eng, reads, writes, pe_accum=False):
        for b in reads:
            self._wait(eng, b.w)
            if b.excl:
                for ev in b.r:
                    if ev[2] != eng:
                        self._wait(eng, ev)
        for b in writes:
            if not (pe_accum and b.w is not None and b.w[2] == "pe" and eng == "pe"):
                self._wait(eng, b.w)
            for ev in b.r:
                self._wait(eng, ev)

    def _commit(self, ev, reads, writes):
        for b in reads:
            b.r.append(ev)
            if len(b.r) > 16:
                best = {}
                for e in b.r:
                    if e[0] not in best or best[e[0]][1] < e[1]:
                        best[e[0]] = e
                b.r = list(best.values())
        for b in writes:
            b.w = ev
            b.r = []

    def op(self, eng, fn, *args, reads=(), writes=(), pe_accum=False, **kw):
        self._deps(eng, reads, writes, pe_accum)
        ins = fn(*args, **kw)
        self.cnt[eng] += 1
        ins.then_inc(self.sems[eng], 1)
        self._commit((eng, self.cnt[eng], eng), reads, writes)
        self.n_ins += 1
        return ins

    def dma(self, q, semkey, out, in_, reads=(), writes=(), **kw):
        self._deps(q, reads, writes)
        ins = self.engs[q].dma_start(out=out, in_=in_, **kw)
        self.cnt[semkey] += 16
        ins.then_inc(self.sems[semkey], 16)
        self._commit((semkey, self.cnt[semkey], "dma"), reads, writes)
        self.n_ins += 1
        return ins

    def barrier(self):
        for e in self.engs:
            for k in self.sems:
                if k != e and self.cnt[k] > 0:
                    self._wait(e, (k, self.cnt[k], None))


class Mem:
    def __init__(self, nc, stack, kb=204):
        self.words = kb * 256
        self.M = stack.enter_context(nc.sbuf_tensor("M", [128, self.words], F32))
        self.top = 0
        self.peak = 0
        self.hist = []

    def _inherit(self, off, end, nb):
        keep = []
        for (a, e, ob) in self.hist:
            if a < end and off < e:
                if ob.w is not None:
                    nb.r.append(ob.w)
                nb.r.extend(ob.r)
                if off <= a and e <= end:
                    continue
            keep.append((a, e, ob))
        best = {}
        for ev in nb.r:
            if ev[0] not in best or best[ev[0]][1] < ev[1]:
                best[ev[0]] = ev
        nb.r = list(best.values())
        keep.append((off, end, nb))
        self.hist = keep

    def mark(self):
        return self.top

    def release(self, m):
        self.top = m

    def alloc(self, name, free_shape, dtype, parts=128):
        n = int(np.prod(free_shape))
        nwords = (n * (2 if dtype == BF16 else 4) + 3) // 4
        off = self.top
        self.top += (nwords + 15) // 16 * 16
        assert self.top <= self.words, f"SBUF overflow at {name}: {self.top * 4} B"
        self.peak = max(self.peak, self.top)
        v = self.M[0:parts, off:off + nwords]
        if dtype == BF16:
            v = v.bitcast(BF16)
        if len(free_shape) > 1:
            names = [f"a{i}" for i in range(len(free_shape))]
            pat = "p (" + " ".join(names) + ") -> p " + " ".join(names)
            v = v.rearrange(pat, **{nm: int(s) for nm, s in zip(names[:-1], free_shape[:-1])})
        nb = Buf(name)
        self._inherit(off, off + (nwords + 15) // 16 * 16, nb)
        return v, nb


def _mem_alloc_top(self, name, free_shape, dtype, parts=128):
    n = int(np.prod(free_shape))
    nwords = (n * (2 if dtype == BF16 else 4) + 3) // 4
    off = self.words - (nwords + 15) // 16 * 16
    v = self.M[0:parts, off:off + nwords]
    if dtype == BF16:
        v = v.bitcast(BF16)
    if len(free_shape) > 1:
        names = [f"a{i}" for i in range(len(free_shape))]
        pat = "p (" + " ".join(names) + ") -> p " + " ".join(names)
        v = v.rearrange(pat, **{nm: int(s) for nm, s in zip(names[:-1], free_shape[:-1])})
    self.top_limit = off
    nb = Buf(name)
    self._inherit(off, self.words, nb)
    return v, nb


Mem.alloc_top = _mem_alloc_top


class Ring:
    def __init__(self, items):
        self.items = items
        self.i = 0

    def next(self):
        it = self.items[self.i % len(self.items)]
        self.i += 1
        return it


def _gammas():
    return 1.0 - np.exp2(-5.0 - np.arange(8, dtype=np.float64))


def host_tables(core):
    f32 = np.float32
    tb = {}
    pos = core * T + np.arange(T, dtype=np.float32)
    invA = (1.0 / (np.float32(500000.0) ** (np.arange(0, 32, 2, dtype=np.float32) / np.float32(32)))).astype(f32)
    invR = (1.0 / (np.float32(10000.0) ** (np.arange(0, 64, 2, dtype=np.float32) / np.float32(64)))).astype(f32)
    angA = (pos[:, None] * invA[None, :]).astype(f32)
    angR = (pos[:, None] * invR[None, :]).astype(f32)
    rope = np.concatenate([np.cos(angA), np.sin(angA), np.cos(angR), np.sin(angR)], axis=1).astype(f32)
    tb["rope"] = np.ascontiguousarray(rope.reshape(NTT, 128, 96).transpose(1, 0, 2)).reshape(128, NTT * 96)
    g = np.zeros((128, 2, 4, 32), f32)
    for j in range(4):
        gb = core * 4 + j
        g[:, 0, j, :] = np.where(np.arange(32) < gb, 0.0, NEGBIG)
        g[:, 1, j, :] = np.where(np.arange(32) < gb, 1.0, 0.0)
    tb["gconst"] = g.reshape(128, 256)
    oh = np.zeros((32, 32, 128), f32)
    for b in range(32):
        oh[b, b, :] = 1.0
    tb["onehot"] = oh.reshape(32, 32 * 128)
    kk = np.arange(128)
    tb["tri"] = np.where(kk[:, None] <= kk[None, :], 0.0, -MASKV).astype(f32)
    tb["ident"] = np.eye(128, dtype=f32)
    gam = _gammas()
    lg = np.log(gam)
    p256 = np.arange(256, dtype=np.float64)
    dec = np.zeros((128, 8, 2, 256), np.float64)
    for h in range(8):
        for mc in range(2):
            m = mc * 128 + np.arange(128)
            diff = p256[None, :] - m[:, None]
            dec[:, h, mc, :] = np.where(diff >= 0, np.exp(lg[h] * np.maximum(diff, 0.0)), 0.0)
    tb["decT"] = dec.astype(f32).reshape(128, 8 * 2 * 256)
    qd = np.zeros((128, 4, 256), np.float64)
    for hp in range(4):
        for par in range(2):
            qd[par * 64:(par + 1) * 64, hp, :] = np.exp(lg[2 * hp + par] * (p256 + 1.0))[None, :]
    tb["qdec"] = qd.astype(f32).reshape(128, 1024)
    kd = np.zeros((128, 2, 8), np.float64)
    for ti in range(2):
        p = ti * 128 + np.arange(128)
        for h in range(8):
            kd[:, ti, h] = np.exp(lg[h] * (255.0 - p))
    tb["kdec"] = kd.astype(f32).reshape(128, 16)
    cd = np.zeros((128, 4), np.float64)
    sc = np.zeros((128, 4, 4), np.float64)
    co = np.zeros((128, 8, 4), np.float64)
    for hp in range(4):
        for par in range(2):
            h = 2 * hp + par
            sl = slice(par * 64, (par + 1) * 64)
            cd[sl, hp] = np.exp(lg[h] * 256.0)
            for i in range(4):
                sc[sl, i, hp] = np.exp(lg[h] * 256.0 * (3 - i))
            for c2 in range(8):
                co[sl, c2, hp] = np.exp(lg[h] * 1024.0 * (core - 1 - c2)) if c2 < core else 0.0
    tb["rsc"] = np.concatenate([cd.reshape(128, 4), sc.reshape(128, 16), co.reshape(128, 32)], axis=1).astype(f32)
    iv = np.zeros((128, 4, 16), f32)
    for gi, w in enumerate((2, 4, 8, 16)):
        n = np.minimum(core * T + np.arange(16) + 1.0, float(w))
        iv[:, gi, :] = (1.0 / n)[None, :]
    tb["invn"] = iv.reshape(128, 64)
    return tb


def feat_vec(v):
    return np.ascontiguousarray(v.reshape(-1, 128).T)


class Prog:
    def __init__(self, mode, dbg=None):
        self.mode = mode
        self.dbg = dbg
        self.nc = bass.Bass("TRN2", target_bir_lowering=False)
        self.ins = {}
        self.outs = {}

    def din(self, name, shape, dt=F32):
        self.ins[name] = (shape, dt)
        return self.nc.dram_tensor(name, list(shape), dt, kind="ExternalInput").ap()

    def dout(self, name, shape, dt=F32):
        self.outs[name] = (shape, dt)
        return self.nc.dram_tensor(name, list(shape), dt, kind="ExternalOutput").ap()

    def dscratch(self, name, shape, dt=F32):
        return self.nc.dram_tensor(name, list(shape), dt, kind="Internal").ap()


def build(mode, dbg=None):
    P = Prog(mode, dbg)
    nc = P.nc
    with contextlib.ExitStack() as st:
        S = Sched(nc, st)
        mem = Mem(nc, st)
        psb = [st.enter_context(nc.psum_tensor(f"ps{i}", [128, 512], F32)) for i in range(8)]
        psB = [Buf(f"ps{i}", excl=True) for i in range(8)]
        PS = [(psb[i][:], psB[i]) for i in range(8)]
        ctx = dict(P=P, nc=nc, S=S, mem=mem, PS=PS, stop=dbg)
        if mode == "A":
            prog_A(ctx)
        else:
            prog_L(ctx)
        S.barrier()
        P.stats = dict(ins=S.n_ins, waits=S.n_wait, peak_kb=mem.peak * 4 / 1024, cnt=dict(S.cnt))
    return P


def mm(S, nc, out, lhsT, rhs, start, stop, reads, writes, **kw):
    return S.op("pe", nc.tensor.matmul, out, lhsT, rhs, start=start, stop=stop,
                reads=reads, writes=writes, pe_accum=True, **kw)


def load_const(ctx, name, dram, shape, dtype, q="sp", parts=128):
    S, mem = ctx["S"], ctx["mem"]
    t, b = mem.alloc(name, shape[1:], dtype, parts=parts)
    sem = S.dma_sem(name)
    src = dram
    if len(shape) > 2:
        names = [f"a{i}" for i in range(len(shape) - 1)]
        pat = "p (" + " ".join(names) + ") -> p " + " ".join(names)
        src = dram.rearrange(pat, **{nm: int(s) for nm, s in zip(names[:-1], shape[1:-1])})
    S.dma(q, sem, t, src, writes=[b])
    return t, b


def inproj_tokmajor(ctx, xT, xTb, w_view, col0, ncols_grp, ngrp, wring, evac):
    S, nc, PS = ctx["S"], ctx["nc"], ctx["PS"]
    psr = ctx["psring"]
    pending = None
    for g in range(ngrp):
        wt, wb, wsem = wring.next()
        S.dma("pool", wsem, wt, w_view[:, :, col0 + g * ncols_grp: col0 + (g + 1) * ncols_grp], writes=[wb])
        for tt in range(NTT):
            ps, pb = psr.next()
            for dc in range(NDC):
                mm(S, nc, ps[:, 0:ncols_grp], xT[:, dc, tt * 128:(tt + 1) * 128], wt[:, dc, :],
                   dc == 0, dc == NDC - 1, [xTb, wb], [pb])
            if pending is not None:
                pending()
            pending = evac(g, tt, ps, pb)
    if pending is not None:
        pending()


def rotary_evac(ctx, ps, pb, dst, dstb, nh, hd, half, cos, sin, ropeb, tmp, tmpb, scale=None):
    S, nc = ctx["S"], ctx["nc"]
    psv = ps[:, 0:nh * hd].rearrange("p (h d) -> p h d", h=nh)
    dv = dst.rearrange("p (h d) -> p h d", h=nh)
    if scale is None:
        S.op("act", nc.scalar.copy, dst, ps[:, 0:nh * hd], reads=[pb], writes=[dstb])
    else:
        S.op("act", nc.scalar.mul, dst, ps[:, 0:nh * hd], scale, reads=[pb], writes=[dstb])
    cb = cos.unsqueeze(1).broadcast_to([128, nh, half])
    sb = sin.unsqueeze(1).broadcast_to([128, nh, half])
    x1 = psv[:, :, 0:half]
    x2 = psv[:, :, half:2 * half]
    t = [tmp[i][:, 0:nh * half].rearrange("p (h d) -> p h d", h=nh) for i in range(4)]
    S.op("dve", nc.vector.tensor_tensor, t[0], x1, cb, ALU.mult, reads=[pb, ropeb], writes=[tmpb[0]])
    S.op("dve", nc.vector.tensor_tensor, t[1], x2, sb, ALU.mult, reads=[pb, ropeb], writes=[tmpb[1]])
    S.op("dve", nc.vector.tensor_tensor, t[2], x1, sb, ALU.mult, reads=[pb, ropeb], writes=[tmpb[2]])
    S.op("dve", nc.vector.tensor_tensor, t[3], x2, cb, ALU.mult, reads=[pb, ropeb], writes=[tmpb[3]])
    if scale is None:
        S.op("dve", nc.vector.tensor_tensor, dv[:, :, 0:half], t[0], t[1], ALU.subtract,
             reads=[tmpb[0], tmpb[1]], writes=[dstb])
        S.op("dve", nc.vector.tensor_tensor, dv[:, :, half:2 * half], t[2], t[3], ALU.add,
             reads=[tmpb[2], tmpb[3]], writes=[dstb])
    else:
        S.op("dve", nc.vector.tensor_tensor, t[0], t[0], t[1], ALU.subtract,
             reads=[tmpb[0], tmpb[1]], writes=[tmpb[0]])
        S.op("dve", nc.vector.tensor_tensor, t[2], t[2], t[3], ALU.add,
             reads=[tmpb[2], tmpb[3]], writes=[tmpb[2]])
        S.op("act", nc.scalar.mul, dv[:, :, 0:half], t[0], scale, reads=[tmpb[0]], writes=[dstb])
        S.op("act", nc.scalar.mul, dv[:, :, half:2 * half], t[2], scale, reads=[tmpb[2]], writes=[dstb])


def common_setup(ctx, need):
    P, nc, S, mem = ctx["P"], ctx["nc"], ctx["S"], ctx["mem"]
    xT_d = P.din("xT", (D, T))
    ctx["xT_d"] = xT_d
    xT, xTb = mem.alloc("xT_bf", (NDC, T), BF16)
    sem = S.dma_sem("xT")
    xv = xT_d.rearrange("(dc p) t -> p dc t", p=128)
    for h in range(4):
        S.dma("pool", sem, xT[:, h * 4:(h + 1) * 4, :], xv[:, h * 4:(h + 1) * 4, :], writes=[xTb] if h == 3 else [])
    ctx["xT"], ctx["xTb"] = xT, xTb
    C = {}
    if "rope" in need:
        C["rope"] = load_const(ctx, "rope", P.din("rope", (128, NTT * 96)), (128, NTT, 96), F32)
    if "ident" in need:
        C["ident"] = load_const(ctx, "ident", P.din("ident", (128, 128)), (128, 128), BF16, q="pool")
    ctx["C"] = C


def transpose_to(ctx, src, srcb, ncol, dst_slices, dstb, identb):
    S, nc = ctx["S"], ctx["nc"]
    ident, ib = identb
    ps, pb = ctx["psring"].next()
    pst = ps[:, 0:256].bitcast(BF16)
    for k in range(ncol):
        S.op("pe", nc.tensor.transpose, pst[:, k * 128:(k + 1) * 128], src[:, k * 128:(k + 1) * 128], ident,
             reads=[srcb, ib], writes=[pb], pe_accum=True)
    return pst, pb


def prog_A(ctx):
    ctx["psring"] = Ring(ctx["PS"])
    common_setup(ctx, ["rope", "ident"])
    rsc, rscb = load_const(ctx, "rsc", ctx["P"].din("rsc", (128, 52)), (128, 52), F32)
    A_body(ctx, rsc, rscb, "")


def A_body(ctx, rsc, rscb, opre):
    P, nc, S, mem, PS = ctx["P"], ctx["nc"], ctx["S"], ctx["mem"], ctx["PS"]
    xT, xTb = ctx["xT"], ctx["xTb"]
    rope, ropeb = ctx["C"]["rope"]
    identb = ctx["C"]["ident"]
    w_a = P.din("w_a", (D, 4608))
    wv = w_a.rearrange("(dc p) n -> p dc n", p=128)
    kdec, kdecb = load_const(ctx, "kdec", P.din("kdec", (128, 16)), (128, 2, 8), F32)
    o_kT = P.dout(opre + "kT_loc", (1024, T), BF16)
    o_v = P.dout(opre + "v_loc", (8 * T, 128), BF16)
    o_kmean = P.dout(opre + "kmean", (128, 32))
    o_krT2 = P.dout(opre + "krT2", (128, 4 * T), BF16)
    o_vr = P.dout(opre + "vr_tok", (128, NTT * 1024), BF16)
    o_kv = P.dout(opre + "kv_loc", (128, 4 * 512))
    o_S = P.dout(opre + "S_loc", (128, 512))
    o_halo = P.dout(opre + "halo", (128, 8 * 16))

    kT, kTb = mem.alloc("kT", (8, T), BF16)
    krT2, krT2b = mem.alloc("krT2", (4, T), BF16)
    ktokd, ktokdb = mem.alloc("ktokd", (NTT, 512), BF16)
    vr, vrb = mem.alloc("vr", (NTT, 1024), BF16)
    wslots = []
    for i in range(2):
        t, b = mem.alloc(f"w{i}", (NDC, 512), BF16)
        wslots.append((t, b, S.dma_sem(f"w{i}")))
    wring = Ring(wslots)
    tokr = []
    for i in range(3):
        t, b = mem.alloc(f"tok{i}", (512,), BF16)
        tokr.append((t, b))
    tokring = Ring(tokr)
    _rt = [mem.alloc(f"rtmp{i}", (256,), F32) for i in range(4)]
    tmp = [a for a, _b in _rt]
    tmpb = [_b for _a, _b in _rt]
    vst = []
    for i in range(2):
        t, b = mem.alloc(f"vst{i}", (512,), BF16)
        vst.append((t, b))
    vring = Ring(vst)
    osem = S.dma_sem("outA")

    def evac_ka(g, tt, ps, pb):
        tk, tkb = tokring.next()
        rotary_evac(ctx, ps, pb, tk, tkb, 4, 128, 16, rope[:, tt, 0:16], rope[:, tt, 16:32], ropeb, tmp, tmpb)
        def post():
            pst, pstb = transpose_to(ctx, tk, tkb, 4, None, None, identb)
            S.op("act", nc.scalar.copy, kT[:, g * 4:(g + 1) * 4, tt * 128:(tt + 1) * 128],
                 pst.rearrange("p (h t) -> p h t", h=4), reads=[pstb], writes=[kTb])
        return post

    inproj_tokmajor(ctx, xT, xTb, wv, 0, 512, 2, wring, evac_ka)

    v_view = o_v.rearrange("(h t) d -> t h d", h=8)

    def evac_va(g, tt, ps, pb):
        t, b = vring.next()
        S.op("act", nc.scalar.copy, t, ps, reads=[pb], writes=[b])
        S.dma("sp", osem, v_view[tt * 128:(tt + 1) * 128, g * 4:(g + 1) * 4, :],
              t.rearrange("p (h d) -> p h d", h=4), reads=[b])

    inproj_tokmajor(ctx, xT, xTb, wv, 1024, 512, 2, wring, evac_va)

    S.dma("sp", osem, o_kT.rearrange("(h p) t -> p h t", p=128), kT, reads=[kTb])
    km, kmb = mem.alloc("km", (8, 4), F32)
    S.op("dve", nc.vector.tensor_reduce, km, kT.rearrange("p h (j t) -> p h j t", j=4), AX.X, ALU.add,
         reads=[kTb], writes=[kmb])
    S.op("act", nc.scalar.mul, km, km, 1.0 / 256.0, reads=[kmb], writes=[kmb])
    S.dma("sp", osem, o_kmean, km.rearrange("p h j -> p (h j)"), reads=[kmb])

    def evac_kr(g, tt, ps, pb):
        tk, tkb = tokring.next()
        rotary_evac(ctx, ps, pb, tk, tkb, 8, 64, 32, rope[:, tt, 32:64], rope[:, tt, 64:96], ropeb, tmp, tmpb,
                    scale=64 ** -0.5)
        S.op("dve", nc.vector.tensor_tensor, ktokd[:, tt, :].rearrange("p (h d) -> p h d", h=8),
             tk.rearrange("p (h d) -> p h d", h=8),
             kdec[:, tt % 2, :].unsqueeze(2).broadcast_to([128, 8, 64]), ALU.mult,
             reads=[tkb, kdecb], writes=[ktokdb])
        def post():
            pst, pstb = transpose_to(ctx, tk, tkb, 4, None, None, identb)
            S.op("act", nc.scalar.copy, krT2[:, :, tt * 128:(tt + 1) * 128],
                 pst.rearrange("p (h t) -> p h t", h=4), reads=[pstb], writes=[krT2b])
        return post

    inproj_tokmajor(ctx, xT, xTb, wv, 2048, 512, 1, wring, evac_kr)

    def evac_vr(g, tt, ps, pb):
        S.op("act", nc.scalar.copy, vr[:, tt, g * 512:(g + 1) * 512], ps, reads=[pb], writes=[vrb])

    inproj_tokmajor(ctx, xT, xTb, wv, 2560, 512, 2, wring, evac_vr)
    S.dma("sp", osem, o_krT2.rearrange("p (h t) -> p h t", h=4), krT2, reads=[krT2b])
    S.dma("sp", osem, o_vr.rearrange("p (a c) -> p a c", a=NTT), vr, reads=[vrb])

    kv, kvb = mem.alloc("kv", (4, 512), F32)
    Sl, Slb = mem.alloc("Sl", (512,), F32)
    for i in range(4):
        ps, pb = ctx["psring"].next()
        for h in range(8):
            hp, par = h // 2, h % 2
            for ti in range(2):
                tt = 2 * i + ti
                mm(S, nc, ps[par * 64:(par + 1) * 64, hp * 128:(hp + 1) * 128],
                   ktokd[:, tt, h * 64:(h + 1) * 64], vr[:, tt, h * 128:(h + 1) * 128],
                   ti == 0, ti == 1, [ktokdb, vrb], [pb], skip_group_check=True)
        S.op("act", nc.scalar.copy, kv[:, i, :], ps, reads=[pb], writes=[kvb])
        for hp in range(4):
            sl = slice(hp * 128, (hp + 1) * 128)
            if i == 0:
                S.op("dve", nc.vector.tensor_scalar_mul, Sl[:, sl], kv[:, i, sl], rsc[:, 4 + i * 4 + hp: 5 + i * 4 + hp],
                     reads=[kvb, rscb], writes=[Slb])
            else:
                S.op("dve", nc.vector.scalar_tensor_tensor, Sl[:, sl], kv[:, i, sl],
                     rsc[:, 4 + i * 4 + hp: 5 + i * 4 + hp], Sl[:, sl], ALU.mult, ALU.add,
                     reads=[kvb, rscb, Slb], writes=[Slb])
    S.dma("sp", osem, o_kv.rearrange("p (i c) -> p i c", i=4), kv, reads=[kvb])
    S.dma("sp", osem, o_S, Sl, reads=[Slb])

    hal, halb = mem.alloc("hal", (8, 16), F32)
    for g in range(2):
        wt, wb, wsem = wring.next()
        S.dma("pool", wsem, wt, wv[:, :, 3584 + g * 512: 3584 + (g + 1) * 512], writes=[wb])
        ps, pb = ctx["psring"].next()
        for cc in range(4):
            for dc in range(NDC):
                mm(S, nc, ps[:, cc * 16:(cc + 1) * 16], wt[:, dc, cc * 128:(cc + 1) * 128], xT[:, dc, T - 16:T],
                   dc == 0, dc == NDC - 1, [xTb, wb], [pb], skip_group_check=True)
        S.op("act", nc.scalar.copy, hal[:, g * 4:(g + 1) * 4, :].rearrange("p a b -> p (a b)"), ps[:, 0:64],
             reads=[pb], writes=[halb])
    S.dma("sp", osem, o_halo, hal.rearrange("p a b -> p (a b)"), reads=[halb])


WL = dict(q_a=0, q_r=1024, g_r=1536, p_in=2560, gate=3584)
WL_COLS = 9728


def wl_from_w_in(w_in_l):
    return np.ascontiguousarray(np.concatenate(
        [w_in_l[:, 0:1024], w_in_l[:, 3072:3584], w_in_l[:, 5120:6144], w_in_l[:, 6144:7168], w_in_l[:, 7168:13312]], axis=1))


def prog_L(ctx):
    P, nc, S, mem, PS = ctx["P"], ctx["nc"], ctx["S"], ctx["mem"], ctx["PS"]
    stop = ctx.get("stop")
    ctx["psring"] = Ring(PS)
    common_setup(ctx, ["rope", "ident"])
    xT, xTb = ctx["xT"], ctx["xTb"]
    xT_d = ctx["xT_d"]
    rope, ropeb = ctx["C"]["rope"]
    identb = ctx["C"]["ident"]
    ident, identB = identb
    w_l = P.din("w_l", (D, WL_COLS))
    wv = w_l.rearrange("(dc p) n -> p dc n", p=128)
    vecs, vecsb = load_const(ctx, "vecs", P.din("vecs", (128, 80)), (128, 80), F32)
    gain = vecs[:, 0:8]
    pscale = vecs[:, 8:16]
    ln1g, ln1b, ln2g, ln2b = vecs[:, 16:32], vecs[:, 32:48], vecs[:, 48:64], vecs[:, 64:80]
    rsc, rscb = load_const(ctx, "rsc", P.din("rsc", (128, 52)), (128, 52), F32)
    cdec = rsc[:, 0:4]
    coef = rsc[:, 20:52]
    epsv, epsb = mem.alloc("epsv", (1,), F32)
    S.op("dve", nc.vector.memset, epsv, LN_EPS, writes=[epsb])
    onesF, onesFb = mem.alloc("onesF", (128,), F32)
    S.op("dve", nc.vector.memset, onesF, 1.0, writes=[onesFb])
    yR_d = P.dout("yR", (1024, T), BF16)
    yA_d = P.dout("yA", (1024, T), BF16)
    yP_d = P.dout("yP", (1024, T), BF16)
    xo_d = P.dout("xT_out", (D, T))
    osem = S.dma_sem("outL")
    yRdb, yAdb, yPdb = Buf("yR_d"), Buf("yA_d"), Buf("yP_d")
    base_mark = mem.mark()

    def wslots_make(n, shape, nm):
        sl = []
        for i in range(n):
            t, b = mem.alloc(f"{nm}{i}", shape, BF16)
            sl.append((t, b, S.dma_sem(f"{nm}{i}")))
        return Ring(sl)

    qrT2, qrT2b = mem.alloc("qrT2", (4, T), BF16)
    qdT2, qdT2b = mem.alloc("qdT2", (4, T), BF16)
    krT2, krT2b = load_const(ctx, "krT2", P.din("krT2", (128, 4 * T), BF16), (128, 4, T), BF16)
    vr, vrb = load_const(ctx, "vr", P.din("vr_tok", (128, NTT * 1024), BF16), (128, NTT, 1024), BF16)
    decT, decTb = load_const(ctx, "decT", P.din("decT", (128, 4096)), (128, 8, 512), BF16, q="pool")
    qdec, qdecb = load_const(ctx, "qdec", P.din("qdec", (128, 1024)), (128, 4, 256), F32)
    kvs, kvsb = load_const(ctx, "kvs", P.din("kv_loc", (128, 2048)), (128, 4, 512), F32)
    Sall_d = P.din("S_all", (8 * 128, 512))
    Sall, Sallb = mem.alloc("Sall", (8, 512), F32)
    S.dma("sp", S.dma_sem("Sall"), Sall, Sall_d.rearrange("(c p) n -> p c n", p=128), writes=[Sallb])
    gs, gsb = mem.alloc("gs", (8, T), BF16)
    yR, yRb = mem.alloc("yR", (8, T), BF16)
    wring = wslots_make(2, (NDC, 512), "wr")
    tokr = Ring([mem.alloc(f"tok{i}", (512,), BF16) for i in range(3)])
    _rt = [mem.alloc(f"rtmp{i}", (256,), F32) for i in range(4)]
    tmp = [a for a, _b in _rt]
    tmpb = [_b for _a, _b in _rt]

    def evac_qr(g, tt, ps, pb):
        tk, tkb = tokr.next()
        rotary_evac(ctx, ps, pb, tk, tkb, 8, 64, 32, rope[:, tt, 32:64], rope[:, tt, 64:96], ropeb, tmp, tmpb)
        def post():
            pst, pstb = transpose_to(ctx, tk, tkb, 4, None, None, identb)
            S.op("act", nc.scalar.copy, qrT2[:, :, tt * 128:(tt + 1) * 128],
                 pst.rearrange("p (h t) -> p h t", h=4), reads=[pstb], writes=[qrT2b])
        return post

    inproj_tokmajor(ctx, xT, xTb, wv, WL["q_r"], 512, 1, wring, evac_qr)
    S.op("dve", nc.vector.tensor_tensor, qdT2.rearrange("p h (i c) -> p h i c", i=4),
         qrT2.rearrange("p h (i c) -> p h i c", i=4),
         qdec.unsqueeze(2).broadcast_to([128, 4, 4, 256]), ALU.mult,
         reads=[qrT2b, qdecb], writes=[qdT2b])
    for g in range(2):
        wt, wb, wsem = wring.next()
        S.dma("pool", wsem, wt, wv[:, :, WL["g_r"] + g * 512: WL["g_r"] + (g + 1) * 512], writes=[wb])
        for hh in range(4):
            for th in range(2):
                ps, pb = ctx["psring"].next()
                for dc in range(NDC):
                    mm(S, nc, ps, wt[:, dc, hh * 128:(hh + 1) * 128], xT[:, dc, th * 512:(th + 1) * 512],
                       dc == 0, dc == NDC - 1, [wb, xTb], [pb])
                S.op("act", nc.scalar.activation, gs[:, g * 4 + hh, th * 512:(th + 1) * 512], ps, AF.Silu,
                     reads=[pb], writes=[gsb])
    prev, prevb = mem.alloc("prev", (512,), F32)
    prevh, prevhb = mem.alloc("prevh", (512,), BF16)
    for c2 in range(8):
        for hp in range(4):
            sl = slice(hp * 128, (hp + 1) * 128)
            sc = coef[:, c2 * 4 + hp: c2 * 4 + hp + 1]
            if c2 == 0:
                S.op("dve", nc.vector.tensor_scalar_mul, prev[:, sl], Sall[:, c2, sl], sc,
                     reads=[Sallb, rscb], writes=[prevb])
            else:
                S.op("dve", nc.vector.scalar_tensor_tensor, prev[:, sl], Sall[:, c2, sl], sc, prev[:, sl],
                     ALU.mult, ALU.add, reads=[Sallb, rscb, prevb], writes=[prevb])
    adr = Ring([mem.alloc(f"adt{i}", (512,), BF16) for i in range(3)])
    f32r = Ring([mem.alloc(f"rf{i}", (256,), F32) for i in range(12)])
    for i in range(4):
        S.op("act", nc.scalar.copy, prevh, prev, reads=[prevb], writes=[prevhb])
        st = {}

        def stage1(h):
            hp, par = h // 2, h % 2
            pp = slice(par * 64, (par + 1) * 64)
            psA, psAb = ctx["psring"].next()
            for mc in range(2):
                mm(S, nc, psA[:, mc * 256:(mc + 1) * 256],
                   krT2[pp, hp, i * 256 + mc * 128: i * 256 + (mc + 1) * 128], qrT2[pp, hp, i * 256:(i + 1) * 256],
                   True, True, [krT2b, qrT2b], [psAb], skip_group_check=True)
            adt, adtb = adr.next()
            S.op("dve", nc.vector.tensor_tensor, adt, psA, decT[:, h, :], ALU.mult,
                 reads=[psAb, decTb], writes=[adtb])
            st[h] = dict(adt=adt, adtb=adtb)

        def stage2(h):
            hp, par = h // 2, h % 2
            pp = slice(par * 64, (par + 1) * 64)
            d = st[h]
            adt, adtb = d["adt"], d["adtb"]
            psR, psRb = ctx["psring"].next()
            mm(S, nc, psR[:, 0:256], vr[:, 2 * i, h * 128:(h + 1) * 128], adt[:, 0:256], True, False, [vrb, adtb], [psRb])
            mm(S, nc, psR[:, 0:256], vr[:, 2 * i + 1, h * 128:(h + 1) * 128], adt[:, 256:512], False, False, [vrb, adtb], [psRb])
            mm(S, nc, psR[:, 0:256], prevh[pp, hp * 128:(hp + 1) * 128], qdT2[pp, hp, i * 256:(i + 1) * 256],
               False, True, [prevhb, qdT2b], [psRb])
            r_sb, r_sbb = f32r.next()
            rsq, rsqb = f32r.next()
            S.op("act", nc.scalar.copy, r_sb, psR[:, 0:256], reads=[psRb], writes=[r_sbb])
            S.op("act", nc.scalar.activation, rsq, psR[:, 0:256], AF.Square, reads=[psRb], writes=[rsqb])
            d.update(r_sb=r_sb, r_sbb=r_sbb, rsq=rsq, rsqb=rsqb)

        def stage3(h):
            d = st.pop(h)
            r_sb, r_sbb, rsq, rsqb = d["r_sb"], d["r_sbb"], d["rsq"], d["rsqb"]
            psM, psMb = ctx["psring"].next()
            mm(S, nc, psM[:, 0:256], onesF, r_sb, True, True, [onesFb, r_sbb], [psMb], skip_group_check=True)
            mm(S, nc, psM[:, 256:512], onesF, rsq, True, True, [onesFb, rsqb], [psMb], skip_group_check=True)
            mean, meanb = f32r.next()
            var, varb = f32r.next()
            S.op("act", nc.scalar.mul, mean, psM[:, 0:256], 1.0 / 128.0, reads=[psMb], writes=[meanb])
            msq, msqb = rsq, rsqb
            S.op("dve", nc.vector.tensor_tensor, msq, mean, mean, ALU.mult, reads=[meanb], writes=[msqb])
            S.op("dve", nc.vector.scalar_tensor_tensor, var, psM[:, 256:512], 1.0 / 128.0, msq, ALU.mult, ALU.subtract,
                 reads=[psMb, msqb], writes=[varb])
            S.op("act", nc.scalar.activation, var, var, AF.Sqrt, bias=epsv, scale=1.0, reads=[varb, epsb], writes=[varb])
            S.op("dve", nc.vector.reciprocal, var, var, reads=[varb], writes=[varb])
            S.op("dve", nc.vector.tensor_tensor, r_sb, r_sb, mean, ALU.subtract, reads=[r_sbb, meanb], writes=[r_sbb])
            S.op("dve", nc.vector.tensor_tensor, r_sb, r_sb, var, ALU.mult, reads=[r_sbb, varb], writes=[r_sbb])
            S.op("dve", nc.vector.scalar_tensor_tensor, yR[:, h, i * 256:(i + 1) * 256], r_sb, gain[:, h:h + 1],
                 gs[:, h, i * 256:(i + 1) * 256], ALU.mult, ALU.mult, reads=[r_sbb, vecsb, gsb], writes=[yRb])

        for step in range(8 + 2):
            if step < 8:
                stage1(step)
            if 0 <= step - 1 < 8:
                stage2(step - 1)
            if 0 <= step - 2 < 8:
                stage3(step - 2)
        if i < 3:
            for hp in range(4):
                sl = slice(hp * 128, (hp + 1) * 128)
                S.op("dve", nc.vector.scalar_tensor_tensor, prev[:, sl], prev[:, sl], cdec[:, hp:hp + 1], kvs[:, i, sl],
                     ALU.mult, ALU.add, reads=[prevb, rscb, kvsb], writes=[prevb])
    S.dma("sp", osem, yR_d.rearrange("(h p) t -> p h t", p=128), yR, reads=[yRb], writes=[yRdb])
    mem.release(base_mark)
    if stop == 1:
        return

    qT, qTb = mem.alloc("qT", (8, T), BF16)
    kTo, kTob = mem.alloc("kTo", (8, T), BF16)
    S.dma("sp", S.dma_sem("kTo"), kTo, P.din("kT_own", (1024, T), BF16).rearrange("(h p) t -> p h t", p=128), writes=[kTob])
    vo_d = P.din("v_own", (8 * T, 128), BF16)
    vo, vob = mem.alloc("vo", (NTT, 8, 128), BF16)
    vosem = S.dma_sem("vo")
    vo_v = vo_d.rearrange("(h tt p) d -> p tt h d", h=8, tt=NTT)
    for tt in range(NTT):
        S.dma("sp", vosem, vo[:, tt], vo_v[:, tt], writes=[vob])
    kmf, kmfb = load_const(ctx, "kmf", P.din("kmean_all", (128, 256)), (128, 8, 32), BF16, q="pool")
    gcon, gconb = load_const(ctx, "gcon", P.din("gconst", (128, 256)), (128, 2, 4, 32), F32)
    onehot_d = P.din("onehot", (32, 4096))
    oneh, onehb = load_const(ctx, "oneh", onehot_d, (32, 32, 128), BF16, q="pool", parts=32)
    tri, trib = load_const(ctx, "tri", P.din("tri", (128, 128)), (128, 128), BF16, q="pool")
    onesB, onesBb = mem.alloc("onesB", (128,), BF16)
    S.op("dve", nc.vector.memset, onesB, 1.0, writes=[onesBb])
    zerB, zerBb = mem.alloc("zerB", (512,), BF16)
    S.op("dve", nc.vector.memset, zerB, 0.0, writes=[zerBb])
    kT_all = P.din("kT_all", (1024, SEQ), BF16)
    v_all = P.din("v_all", (8 * SEQ, 128), BF16)
    wring = wslots_make(2, (NDC, 512), "wa")
    tokr = Ring([mem.alloc(f"tok{i}", (512,), BF16) for i in range(3)])
    _rt = [mem.alloc(f"rtmpa{i}", (256,), F32) for i in range(4)]
    tmp = [a for a, _b in _rt]
    tmpb = [_b for _a, _b in _rt]

    def evac_qa(g, tt, ps, pb):
        tk, tkb = tokr.next()
        rotary_evac(ctx, ps, pb, tk, tkb, 4, 128, 16, rope[:, tt, 0:16], rope[:, tt, 16:32], ropeb, tmp, tmpb)
        def post():
            pst, pstb = transpose_to(ctx, tk, tkb, 4, None, None, identb)
            S.op("act", nc.scalar.copy, qT[:, g * 4:(g + 1) * 4, tt * 128:(tt + 1) * 128],
                 pst.rearrange("p (h t) -> p h t", h=4), reads=[pstb], writes=[qTb])
        return post

    inproj_tokmajor(ctx, xT, xTb, wv, WL["q_a"], 512, 2, wring, evac_qa)
    att_mark = mem.mark()
    kbr = []
    vbr = []
    for i in range(2):
        t, b = mem.alloc(f"kb{i}", (4096,), BF16)
        kbr.append((t, b, S.dma_sem(f"kb{i}")))
        t, b = mem.alloc(f"vb{i}", (32, 128), BF16)
        vbr.append((t, b, S.dma_sem(f"vb{i}")))
    kbr, vbr = Ring(kbr), Ring(vbr)
    mskr = Ring([mem.alloc(f"msk{i}", (T,), BF16, parts=32) for i in range(2)])
    ptr = Ring([mem.alloc(f"pt{i}", (512,), BF16) for i in range(6)])
    gtmp = Ring([mem.alloc(f"gt{i}", (32,), F32) for i in range(4)])
    mx8, mx8b = mem.alloc("mx8", (8,), F32)
    mbb = Ring([mem.alloc(f"mb{i}", (32,), BF16) for i in range(2)])
    yAr = Ring([mem.alloc(f"yAh{i}", (T,), BF16) for i in range(2)])
    rsr = Ring([mem.alloc(f"rs{i}", (512,), F32) for i in range(2)])
    psO = PS[0:4]
    psS = Ring(PS[4:8])
    gate_state = {}

    def gate_pre(hh, tt, msk_t):
        j = tt // 2
        psg, psgb = psS.next()
        mm(S, nc, psg[:, 0:32], qT[:, hh, tt * 128:(tt + 1) * 128], kmf[:, hh, :], True, True, [qTb, kmfb], [psgb])
        gm, gmb = gtmp.next()
        S.op("dve", nc.vector.tensor_tensor, gm, psg[:, 0:32], gcon[:, 0, j, :], ALU.add,
             reads=[psgb, gconb], writes=[gmb])
        S.op("dve", nc.vector.max, mx8, gm, reads=[gmb], writes=[mx8b])
        sel, selb = gtmp.next()
        S.op("dve", nc.vector.tensor_single_scalar, sel, gm, mx8[:, 2:3], ALU.is_ge,
             reads=[gmb, mx8b], writes=[selb])
        S.op("dve", nc.vector.tensor_tensor, sel, sel, gcon[:, 1, j, :], ALU.mult,
             reads=[selb, gconb], writes=[selb])
        mb, mbB = mbb.next()
        S.op("dve", nc.vector.tensor_scalar, mb, sel, 1.0, MASKV, ALU.subtract, ALU.mult,
             reads=[selb], writes=[mbB])
        gate_state[(hh, tt)] = (mb, mbB)

    def gate_post(hh, tt, msk_t):
        msk_, mskb_ = msk_t
        mb, mbB = gate_state.pop((hh, tt))
        pst_, pstb_ = psS.next()
        pstv = pst_[:, 0:64].bitcast(BF16)
        S.op("pe", nc.tensor.transpose, pstv[0:32, 0:128], mb, ident, reads=[mbB, identB], writes=[pstb_], pe_accum=True)
        S.op("act", nc.scalar.copy, msk_[0:32, tt * 128:(tt + 1) * 128], pstv[0:32, 0:128], reads=[pstb_], writes=[mskb_])

    msk_next = mskr.next()
    for tt in range(NTT):
        gate_pre(0, tt, msk_next)
        gate_post(0, tt, msk_next)
    for h in range(8):
        msk, mskb = msk_next
        if h < 7:
            msk_next = mskr.next()
        for j in range(4):
            mm(S, nc, psO[j][0], zerB[:, 0:128], zerB, True, False, [zerBb], [psO[j][1]], skip_group_check=True)
        items = []
        for half in range(2):
            kb, kbB, kbsem = kbr.next()
            vb, vbB, vbsem = vbr.next()
            S.dma("sp", kbsem, kb, kT_all[h * 128:(h + 1) * 128, half * 4096:(half + 1) * 4096], writes=[kbB])
            S.dma("act", vbsem, vb,
                  v_all[h * SEQ + half * 4096: h * SEQ + (half + 1) * 4096, :].rearrange("(kc p) d -> p kc d", p=128),
                  writes=[vbB])
            for g2 in range(2):
                for kc in range(32):
                    blk = (half * 32 + kc) // 2
                    items.append(dict(kind="g", g2=g2, nq=512, q0=g2 * 512, o0=0,
                                      lhs=kb[:, kc * 128:(kc + 1) * 128], lhsb=kbB, blk=blk,
                                      v=vb[:, kc, :], vbuf=vbB, last=False))
        for j in range(4):
            items.append(dict(kind="o", g2=j // 2, nq=256, q0=j * 256, o0=(j % 2) * 256,
                              lhs=kTo[:, h, j * 256: j * 256 + 128], lhsb=kTob,
                              v=vo[:, 2 * j, h, :], vbuf=vob, last=False))
            items.append(dict(kind="o", g2=j // 2, nq=128, q0=j * 256 + 128, o0=(j % 2) * 256 + 128,
                              lhs=kTo[:, h, j * 256 + 128:(j + 1) * 256], lhsb=kTob,
                              v=vo[:, 2 * j + 1, h, :], vbuf=vob, last=True))

        def emit_qk(it):
            pss, pssb = psS.next()
            nq = it["nq"]
            mm(S, nc, pss[:, 0:nq], it["lhs"], qT[:, h, it["q0"]:it["q0"] + nq], True, False, [it["lhsb"], qTb], [pssb])
            if it["kind"] == "g":
                mm(S, nc, pss[:, 0:nq], oneh[0:32, it["blk"], :], msk[0:32, it["q0"]:it["q0"] + nq], False, True,
                   [onehb, mskb], [pssb])
            else:
                mm(S, nc, pss[:, 0:128], ident, tri, False, True, [identB, trib], [pssb], skip_group_check=True)
            pt, ptb = ptr.next()
            S.op("act", nc.scalar.activation, pt[:, 0:nq], pss[:, 0:nq], AF.Exp, scale=ATT_SCALE,
                 reads=[pssb], writes=[ptb])
            it["pt"], it["ptb"] = pt, ptb

        def emit_pv(it):
            g2, nq, o0 = it["g2"], it["nq"], it["o0"]
            pt, ptb = it["pt"], it["ptb"]
            mm(S, nc, psO[2 * g2][0][:, o0:o0 + nq], it["v"], pt[:, 0:nq], False, it["last"], [it["vbuf"], ptb],
               [psO[2 * g2][1]], skip_group_check=True)
            mm(S, nc, psO[2 * g2 + 1][0][:, o0:o0 + nq], onesB, pt[:, 0:nq], False, it["last"], [onesBb, ptb],
               [psO[2 * g2 + 1][1]], skip_group_check=True)

        PDEPTH = 3
        for idx, it in enumerate(items):
            emit_qk(it)
            if idx >= PDEPTH:
                emit_pv(items[idx - PDEPTH])
            if h < 7 and idx >= 8 and (idx - 8) % 16 == 0 and (idx - 8) // 16 < NTT:
                gate_pre(h + 1, (idx - 8) // 16, msk_next)
            if h < 7 and idx >= 16 and (idx - 16) % 16 == 0 and (idx - 16) // 16 < NTT:
                gate_post(h + 1, (idx - 16) // 16, msk_next)
        for it in items[len(items) - PDEPTH:]:
            emit_pv(it)
        yAh, yAhb = yAr.next()
        for g2 in range(2):
            rs, rsb = rsr.next()
            S.op("dve", nc.vector.reciprocal, rs, psO[2 * g2 + 1][0], reads=[psO[2 * g2 + 1][1]], writes=[rsb])
            S.op("dve", nc.vector.tensor_tensor, yAh[:, g2 * 512:(g2 + 1) * 512], psO[2 * g2][0], rs, ALU.mult,
                 reads=[psO[2 * g2][1], rsb], writes=[yAhb])
        S.dma("sp", osem, yA_d[h * 128:(h + 1) * 128, :], yAh, reads=[yAhb], writes=[yAdb])
    mem.release(base_mark)
    if stop == 2:
        return
    prog_L2(ctx, locals())


def layer_norm_fm(ctx, acc, accb, gcol, bcol, vecsb, onesF, onesFb, epsv, epsb, hT=None, hTb=None):
    S, nc, mem = ctx["S"], ctx["nc"], ctx["mem"]
    mk = mem.mark()
    sqr = Ring([mem.alloc(f"lnsq{i}", (256,), F32) for i in range(2)])
    mean, meanb = mem.alloc("lnmean", (256,), F32)
    rstd, rstdb = mem.alloc("lnrstd", (256,), F32)
    msq, msqb = mem.alloc("lnmsq", (256,), F32)
    psr = ctx["psring"]
    for th in range(4):
        ts = slice(th * 256, (th + 1) * 256)
        psM, psMb = psr.next()
        psV, psVb = psr.next()
        for oc in range(NDC):
            mm(S, nc, psM[:, 0:256], onesF, acc[:, oc, ts], oc == 0, oc == NDC - 1, [onesFb, accb], [psMb])
        for oc in range(NDC):
            sq, sqb = sqr.next()
            S.op("act", nc.scalar.activation, sq, acc[:, oc, ts], AF.Square, reads=[accb], writes=[sqb])
            mm(S, nc, psV[:, 0:256], onesF, sq, oc == 0, oc == NDC - 1, [onesFb, sqb], [psVb])
        S.op("act", nc.scalar.mul, mean, psM[:, 0:256], 1.0 / D, reads=[psMb], writes=[meanb])
        S.op("dve", nc.vector.tensor_tensor, msq, mean, mean, ALU.mult, reads=[meanb], writes=[msqb])
        S.op("dve", nc.vector.scalar_tensor_tensor, rstd, psV[:, 0:256], 1.0 / D, msq, ALU.mult, ALU.subtract,
             reads=[psVb, msqb], writes=[rstdb])
        S.op("act", nc.scalar.activation, rstd, rstd, AF.Sqrt, bias=epsv, scale=1.0, reads=[rstdb, epsb], writes=[rstdb])
        S.op("dve", nc.vector.reciprocal, rstd, rstd, reads=[rstdb], writes=[rstdb])
        for oc in range(NDC):
            a = acc[:, oc, ts]
            S.op("dve", nc.vector.tensor_tensor, a, a, mean, ALU.subtract, reads=[accb, meanb], writes=[accb])
            S.op("dve", nc.vector.tensor_tensor, a, a, rstd, ALU.mult, reads=[accb, rstdb], writes=[accb])
            S.op("dve", nc.vector.tensor_scalar, a, a, gcol[:, oc:oc + 1], bcol[:, oc:oc + 1], ALU.mult, ALU.add,
                 reads=[accb, vecsb], writes=[accb])
            if hT is not None:
                S.op("act", nc.scalar.copy, hT[:, oc, ts], a, reads=[accb], writes=[hTb])
    mem.release(mk)


def prog_L2(ctx, L):
    P, nc, S, mem, PS = ctx["P"], ctx["nc"], ctx["S"], ctx["mem"], ctx["PS"]
    stop = ctx.get("stop")
    xT, xTb, wv, vecs, vecsb = L["xT"], L["xTb"], L["wv"], L["vecs"], L["vecsb"]
    yR_d, yA_d, yP_d, xo_d, osem, xT_d = L["yR_d"], L["yA_d"], L["yP_d"], L["xo_d"], L["osem"], L["xT_d"]
    base_mark = L["base_mark"]
    onesF, onesFb, epsv, epsb = L["onesF"], L["onesFb"], L["epsv"], L["epsb"]
    pscale = L["pscale"]
    ctx["psring"] = Ring(PS)
    psr = ctx["psring"]

    def wslots_make(n, shape, nm):
        sl = []
        for i in range(n):
            t, b = mem.alloc(f"{nm}{i}", shape, BF16)
            sl.append((t, b, S.dma_sem(f"{nm}{i}")))
        return Ring(sl)

    pext, pextb = mem.alloc("pext", (8, 16 + T), F32)
    S.dma("sp", S.dma_sem("halo"), pext[:, :, 0:16], P.din("halo_in", (128, 128)).rearrange("p (a b) -> p a b", a=8),
          writes=[pextb])
    invn, invnb = load_const(ctx, "invn", P.din("invn", (128, 64)), (128, 4, 16), F32)
    wp, wpb = mem.alloc("wp", (4, 2, 256), BF16)
    S.dma("pool", S.dma_sem("wp"), wp, P.din("w_pool", (1024, 256)).rearrange("(g cc p) e -> p g cc e", g=4, cc=2),
          writes=[wpb])
    wring = wslots_make(2, (NDC, 512), "wpi")
    for g in range(2):
        wt, wb, wsem = wring.next()
        S.dma("pool", wsem, wt, wv[:, :, WL["p_in"] + g * 512: WL["p_in"] + (g + 1) * 512], writes=[wb])
        for cc in range(4):
            for th in range(2):
                ps, pb = psr.next()
                for dc in range(NDC):
                    mm(S, nc, ps, wt[:, dc, cc * 128:(cc + 1) * 128], xT[:, dc, th * 512:(th + 1) * 512],
                       dc == 0, dc == NDC - 1, [wb, xTb], [pb])
                S.op("act", nc.scalar.copy, pext[:, g * 4 + cc, 16 + th * 512: 16 + (th + 1) * 512], ps,
                     reads=[pb], writes=[pextb])
    sA, sAb = mem.alloc("sA", (2, 16 + T), F32)
    sB, sBb = mem.alloc("sB", (2, 16 + T), F32)
    dT, dTb = mem.alloc("dT", (2, T), BF16)
    yP, yPb = mem.alloc("yP", (8, T), BF16)
    NX = 16 + T
    for gi, w in enumerate((2, 4, 8, 16)):
        src, srcb = pext[:, 2 * gi:2 * gi + 2, :], pextb
        k = 1
        bufs = [(sA, sAb), (sB, sBb)]
        bi = 0
        while k < w:
            dst, dstb = bufs[bi]
            S.op("dve", nc.vector.tensor_tensor, dst[:, :, k:NX], src[:, :, k:NX], src[:, :, 0:NX - k], ALU.add,
                 reads=[srcb], writes=[dstb])
            if k > 1 or True:
                pass
            src, srcb = dst, dstb
            bi ^= 1
            k *= 2
        S.op("dve", nc.vector.scalar_tensor_tensor, dT, src[:, :, 16:NX], 1.0 / w, pext[:, 2 * gi:2 * gi + 2, 16:NX],
             ALU.mult, ALU.subtract, reads=[srcb, pextb], writes=[dTb])
        tmp16, tmp16b = bufs[bi]
        S.op("dve", nc.vector.tensor_tensor, tmp16[:, :, 0:16], src[:, :, 16:32],
             invn[:, gi, :].unsqueeze(1).broadcast_to([128, 2, 16]), ALU.mult, reads=[srcb, invnb], writes=[tmp16b])
        S.op("dve", nc.vector.tensor_tensor, dT[:, :, 0:16], tmp16[:, :, 0:16], pext[:, 2 * gi:2 * gi + 2, 16:32],
             ALU.subtract, reads=[tmp16b, pextb], writes=[dTb])
        for ec in range(2):
            for th in range(2):
                ps, pb = psr.next()
                for cc in range(2):
                    mm(S, nc, ps, wp[:, gi, cc, ec * 128:(ec + 1) * 128], dT[:, cc, th * 512:(th + 1) * 512],
                       cc == 0, cc == 1, [wpb, dTb], [pb])
                ch = 2 * gi + ec
                S.op("act", nc.scalar.activation, yP[:, ch, th * 512:(th + 1) * 512], ps, AF.Copy,
                     scale=pscale[:, ch:ch + 1], reads=[pb, vecsb], writes=[yPb])
    S.dma("sp", osem, yP_d.rearrange("(h p) t -> p h t", p=128), yP, reads=[yPb], writes=[L["yPdb"]])
    mem.release(base_mark)
    if stop == 3:
        return

    mem.peak = mem.top
    merged, mergedb = mem.alloc_top("merged", (NDC, T), BF16)
    br, brb = mem.alloc("br", (3, 8, T), BF16)
    bsem = S.dma_sem("br")
    for n, (d_, db_) in enumerate(((yA_d, L["yAdb"]), (yR_d, L["yRdb"]), (yP_d, L["yPdb"]))):
        S.dma("sp", bsem, br[:, n], d_.rearrange("(h p) t -> p h t", p=128), reads=[db_], writes=[brb])
    w_br = P.din("w_branch", (3 * 1024, D))
    wbv = w_br.rearrange("(n kc p) c -> p n kc c", n=3, kc=8)
    wgr = wslots_make(2, (3, NDC, 128), "wg")
    wbr = wslots_make(2, (3, 8, 128), "wb")
    sgr = Ring([mem.alloc(f"sg{i}", (512,), F32) for i in range(3)])
    mtr = Ring([mem.alloc(f"mt{i}", (512,), F32) for i in range(3)])
    for dch in range(NDC):
        wg, wgb, wgsem = wgr.next()
        wb_, wbb, wbsem = wbr.next()
        for n in range(3):
            c0 = WL["gate"] + n * D + dch * 128
            S.dma("pool", wgsem, wg[:, n], wv[:, :, c0:c0 + 128], writes=[wgb])
            S.dma("pool", wbsem, wb_[:, n], wbv[:, n, :, dch * 128:(dch + 1) * 128], writes=[wbb])
        for th in range(2):
            ts = slice(th * 512, (th + 1) * 512)
            prods = []
            for n in range(3):
                psG, psGb = psr.next()
                for dc in range(NDC):
                    mm(S, nc, psG, wg[:, n, dc, :], xT[:, dc, ts], dc == 0, dc == NDC - 1, [wgb, xTb], [psGb])
                psB_, psBb = psr.next()
                for kc in range(8):
                    mm(S, nc, psB_, wb_[:, n, kc, :], br[:, n, kc, ts], kc == 0, kc == 7, [wbb, brb], [psBb])
                sg, sgb = sgr.next()
                S.op("act", nc.scalar.activation, sg, psG, AF.Sigmoid, reads=[psGb], writes=[sgb])
                S.op("dve", nc.vector.tensor_tensor, sg, sg, psB_, ALU.mult, reads=[sgb, psBb], writes=[sgb])
                prods.append((sg, sgb))
            mt, mtb = mtr.next()
            S.op("dve", nc.vector.tensor_tensor, mt, prods[0][0], prods[1][0], ALU.add,
                 reads=[prods[0][1], prods[1][1]], writes=[mtb])
            S.op("dve", nc.vector.tensor_tensor, merged[:, dch, ts], mt, prods[2][0], ALU.add,
                 reads=[mtb, prods[2][1]], writes=[mergedb])
    if stop == 4:
        S.dma("sp", osem, xo_d.rearrange("(c p) t -> p c t", p=128)[:, :, 0:512],
              merged.bitcast(F32), reads=[mergedb])
        return
    assert mem.peak <= mem.top_limit, (mem.peak, mem.top_limit)
    mem.release(base_mark)

    acc, accb = mem.alloc("acc", (NDC, T), F32)
    hT, hTb = xT, xTb
    mk_o = mem.mark()
    w_out = P.din("w_out", (D, D))
    wov = w_out.rearrange("(kc p) c -> p kc c", p=128)
    wor = wslots_make(2, (NDC, 128), "wo")
    xrr = []
    for i in range(3):
        t, b = mem.alloc(f"xr{i}", (512,), F32)
        xrr.append((t, b, S.dma_sem(f"xr{i}")))
    xrr = Ring(xrr)
    xTv = xT_d.rearrange("(c p) t -> p c t", p=128)
    for oc in range(NDC):
        wo, wob, wosem = wor.next()
        S.dma("pool", wosem, wo, wov[:, :, oc * 128:(oc + 1) * 128], writes=[wob])
        for th in range(2):
            ts = slice(th * 512, (th + 1) * 512)
            xr, xrb, xrsem = xrr.next()
            S.dma("sp", xrsem, xr, xTv[:, oc, ts], writes=[xrb])
            ps, pb = psr.next()
            for kc in range(NDC):
                mm(S, nc, ps, wo[:, kc, :], merged[:, kc, ts], kc == 0, kc == NDC - 1, [wob, mergedb], [pb])
            S.op("dve", nc.vector.scalar_tensor_tensor, acc[:, oc, ts], xr, ALPHA, ps, ALU.mult, ALU.add,
                 reads=[xrb, pb], writes=[accb])
    mem.release(mk_o)
    if stop == 45:
        S.dma("sp", osem, xo_d.rearrange("(c p) t -> p c t", p=128), acc, reads=[accb])
        return
    layer_norm_fm(ctx, acc, accb, L["ln1g"], L["ln1b"], vecsb, onesF, onesFb, epsv, epsb, hT, hTb)
    if stop == 5:
        S.dma("sp", osem, xo_d.rearrange("(c p) t -> p c t", p=128), acc, reads=[accb])
        return
    prog_MoE(ctx, L, acc, accb, hT, hTb)
    with_A = ctx["P"].mode == "LA"
    layer_norm_fm(ctx, acc, accb, L["ln2g"], L["ln2b"], vecsb, onesF, onesFb, epsv, epsb,
                  hT if with_A else None, hTb if with_A else None)
    S.dma("sp", osem, xo_d.rearrange("(c p) t -> p c t", p=128), acc, reads=[accb])
    if with_A:
        mem.release(base_mark)
        ctx["psring"] = Ring(PS)
        A_body(ctx, L["rsc"], L["rscb"], "o_")


def prog_MoE(ctx, L, acc, accb, hT, hTb):
    P, nc, S, mem, PS = ctx["P"], ctx["nc"], ctx["S"], ctx["mem"], ctx["PS"]
    psr = ctx["psring"]
    osem = L["osem"]
    mk = mem.mark()
    wr, wrb = mem.alloc("wr", (NDC, 36), F32)
    S.dma("sp", S.dma_sem("wr"), wr, P.din("w_r", (D, 36)).rearrange("(dc p) n -> p dc n", p=128), writes=[wrb])
    brr, brrb = load_const(ctx, "brr", P.din("b_r", (128, 36)), (128, 36), F32)
    oneh, onehb = mem.alloc("oneh2", (32, 128), BF16, parts=32)
    S.dma("pool", S.dma_sem("oneh2"), oneh, L["onehot_d"].rearrange("p (a b) -> p a b", a=32), writes=[onehb])
    ident, identB = ctx["C"]["ident"]
    whi, whib = mem.alloc("whi", (T,), BF16, parts=32)
    wlo, wlob = mem.alloc("wlo", (T,), BF16, parts=32)
    rt = {}
    for nm, n in [("lg", 36), ("m1", 1), ("nm1", 1), ("e1", 4), ("s1", 1), ("gtop", 1), ("gmask", 4), ("gpen", 4),
                  ("l2m", 32), ("mx8", 8), ("nmx", 1), ("e2", 1), ("den", 1), ("wA", 1), ("wB", 1), ("t1", 32),
                  ("t2", 32), ("wf", 32), ("hif", 32)]:
        rt[nm] = mem.alloc("r_" + nm, (n,), F32)
    hi_, hib = mem.alloc("r_hi", (32,), BF16)
    lo_, lob = mem.alloc("r_lo", (32,), BF16)

    def V(op, out, *args, **kw):
        return S.op("dve", op, out[0], *args, writes=[out[1]], **kw)

    for tt in range(NTT):
        psR, psRb = psr.next()
        for dc in range(NDC):
            mm(S, nc, psR[:, 0:36], acc[:, dc, tt * 128:(tt + 1) * 128], wr[:, dc, :], dc == 0, dc == NDC - 1,
               [accb, wrb], [psRb])
        lg, lgb = rt["lg"]
        V(nc.vector.tensor_tensor, rt["lg"], psR[:, 0:36], brr, ALU.add, reads=[psRb, brrb])
        V(nc.vector.tensor_reduce, rt["m1"], lg[:, 0:4], AX.X, ALU.max, reads=[lgb])
        V(nc.vector.tensor_single_scalar, rt["nm1"], rt["m1"][0], -1.0, ALU.mult, reads=[rt["m1"][1]])
        S.op("act", nc.scalar.activation, rt["e1"][0], lg[:, 0:4], AF.Exp, bias=rt["nm1"][0], scale=1.0,
             accum_out=rt["s1"][0], reads=[lgb, rt["nm1"][1]], writes=[rt["e1"][1], rt["s1"][1]])
        V(nc.vector.reciprocal, rt["gtop"], rt["s1"][0], reads=[rt["s1"][1]])
        V(nc.vector.tensor_single_scalar, rt["gmask"], lg[:, 0:4], rt["m1"][0], ALU.is_ge, reads=[lgb, rt["m1"][1]])
        V(nc.vector.tensor_scalar, rt["gpen"], rt["gmask"][0], 1.0, 1e30, ALU.subtract, ALU.mult, reads=[rt["gmask"][1]])
        l2m, l2mb = rt["l2m"]
        S.op("dve", nc.vector.tensor_tensor, l2m.rearrange("p (g j) -> p g j", g=4),
             lg[:, 4:36].rearrange("p (g j) -> p g j", g=4),
             rt["gpen"][0].unsqueeze(2).broadcast_to([128, 4, 8]), ALU.add,
             reads=[lgb, rt["gpen"][1]], writes=[l2mb])
        mx8, mx8b = rt["mx8"]
        V(nc.vector.max, rt["mx8"], l2m, reads=[l2mb])
        V(nc.vector.tensor_single_scalar, rt["nmx"], mx8[:, 0:1], -1.0, ALU.mult, reads=[mx8b])
        S.op("act", nc.scalar.activation, rt["e2"][0], mx8[:, 1:2], AF.Exp, bias=rt["nmx"][0], scale=1.0,
             reads=[mx8b, rt["nmx"][1]], writes=[rt["e2"][1]])
        V(nc.vector.tensor_single_scalar, rt["den"], rt["e2"][0], 1.0, ALU.add, reads=[rt["e2"][1]])
        V(nc.vector.reciprocal, rt["den"], rt["den"][0], reads=[rt["den"][1]])
        V(nc.vector.tensor_tensor, rt["wA"], rt["gtop"][0], rt["den"][0], ALU.mult, reads=[rt["gtop"][1], rt["den"][1]])
        V(nc.vector.tensor_tensor, rt["wB"], rt["wA"][0], rt["e2"][0], ALU.mult, reads=[rt["wA"][1], rt["e2"][1]])
        V(nc.vector.tensor_scalar, rt["t1"], l2m, mx8[:, 0:1], rt["wA"][0], ALU.is_equal, ALU.mult,
          reads=[l2mb, mx8b, rt["wA"][1]])
        V(nc.vector.tensor_scalar, rt["t2"], l2m, mx8[:, 1:2], rt["wB"][0], ALU.is_equal, ALU.mult,
          reads=[l2mb, mx8b, rt["wB"][1]])
        V(nc.vector.tensor_tensor, rt["wf"], rt["t1"][0], rt["t2"][0], ALU.add, reads=[rt["t1"][1], rt["t2"][1]])
        S.op("act", nc.scalar.copy, hi_, rt["wf"][0], reads=[rt["wf"][1]], writes=[hib])
        S.op("dve", nc.vector.tensor_tensor, lo_, rt["wf"][0], hi_, ALU.subtract, reads=[rt["wf"][1], hib], writes=[lob])
        for src, srcb, dst, dstb in ((hi_, hib, whi, whib), (lo_, lob, wlo, wlob)):
            pst_, pstb_ = psr.next()
            pstv = pst_[:, 0:64].bitcast(BF16)
            S.op("pe", nc.tensor.transpose, pstv[0:32, 0:128], src, ident, reads=[srcb, identB], writes=[pstb_], pe_accum=True)
            S.op("act", nc.scalar.copy, dst[0:32, tt * 128:(tt + 1) * 128], pstv[0:32, 0:128], reads=[pstb_], writes=[dstb])
    for oc in range(NDC):
        S.op("act", nc.scalar.mul, acc[:, oc, :], acc[:, oc, :], ALPHA, reads=[accb], writes=[accb])
    if ctx.get("stop") == 6:
        return

    def wslots(n, shape, nm):
        sl = []
        for i in range(n):
            t, b = mem.alloc(f"{nm}{i}", shape, BF16)
            sl.append((t, b, S.dma_sem(f"{nm}{i}")))
        return Ring(sl)

    wgur = wslots(4, (NDC, 256), "wgu")
    wdr = wslots(2, (4, 1024), "wd")
    aT, aTb = mem.alloc("aT", (4, T), BF16)
    wbcr = Ring([mem.alloc(f"wbc{i}", (T,), F32) for i in range(2)])
    sgr = Ring([mem.alloc(f"msg{i}", (512,), F32) for i in range(2)])
    uwr = Ring([mem.alloc(f"muw{i}", (512,), F32) for i in range(2)])
    w_eg = P.din("w_eg", (32 * D, 512)).rearrange("(e dc p) f -> e p dc f", e=32, p=128)
    w_eu = P.din("w_eu", (32 * D, 512)).rearrange("(e dc p) f -> e p dc f", e=32, p=128)
    w_ed = P.din("w_ed", (32 * 512, D)).rearrange("(e fc p) o -> e p fc o", e=32, p=128)
    n_exp = int(ctx.get("n_exp", 32))
    for e in range(n_exp):
        wbc, wbcb = wbcr.next()
        for th in range(2):
            ts = slice(th * 512, (th + 1) * 512)
            ps, pb = psr.next()
            mm(S, nc, ps, oneh[0:32, e, :], whi[0:32, ts], True, False, [onehb, whib], [pb])
            mm(S, nc, ps, oneh[0:32, e, :], wlo[0:32, ts], False, True, [onehb, wlob], [pb])
            S.op("act", nc.scalar.copy, wbc[:, ts], ps, reads=[pb], writes=[wbcb])
        for fh in range(2):
            wg, wgb, wgsem = wgur.next()
            wu, wub, wusem = wgur.next()
            S.dma("pool", wgsem, wg, w_eg[e][:, :, fh * 256:(fh + 1) * 256], writes=[wgb])
            S.dma("pool", wusem, wu, w_eu[e][:, :, fh * 256:(fh + 1) * 256], writes=[wub])
            for fq in range(2):
                fc = fh * 2 + fq
                for th in range(2):
                    ts = slice(th * 512, (th + 1) * 512)
                    psG, psGb = psr.next()
                    for dc in range(NDC):
                        mm(S, nc, psG, wg[:, dc, fq * 128:(fq + 1) * 128], hT[:, dc, ts], dc == 0, dc == NDC - 1,
                           [wgb, hTb], [psGb])
                    psU, psUb = psr.next()
                    for dc in range(NDC):
                        mm(S, nc, psU, wu[:, dc, fq * 128:(fq + 1) * 128], hT[:, dc, ts], dc == 0, dc == NDC - 1,
                           [wub, hTb], [psUb])
                    sg, sgb = sgr.next()
                    uw, uwb = uwr.next()
                    S.op("act", nc.scalar.activation, sg, psG, AF.Silu, reads=[psGb], writes=[sgb])
                    S.op("dve", nc.vector.tensor_tensor, uw, psU, wbc[:, ts], ALU.mult, reads=[psUb, wbcb], writes=[uwb])
                    S.op("dve", nc.vector.tensor_tensor, aT[:, fc, ts], sg, uw, ALU.mult, reads=[sgb, uwb], writes=[aTb])
        for oh in range(2):
            wd, wdb, wdsem = wdr.next()
            S.dma("pool", wdsem, wd, w_ed[e][:, :, oh * 1024:(oh + 1) * 1024], writes=[wdb])
            for oq in range(8):
                oc = oh * 8 + oq
                for th in range(2):
                    ts = slice(th * 512, (th + 1) * 512)
                    psY, psYb = psr.next()
                    for fc in range(4):
                        mm(S, nc, psY, wd[:, fc, oq * 128:(oq + 1) * 128], aT[:, fc, ts], fc == 0, fc == 3,
                           [wdb, aTb], [psYb])
                    S.op("dve", nc.vector.tensor_tensor, acc[:, oc, ts], acc[:, oc, ts], psY, ALU.add,
                         reads=[accb, psYb], writes=[accb])
    mem.release(mk)


def layer_inputs(inp, l):
    w_in = inp["w_in"][l]
    m = {}
    m["w_l"] = wl_from_w_in(w_in)
    m["w_a"] = np.ascontiguousarray(np.concatenate([w_in[:, 1024:3072], w_in[:, 3584:5120], w_in[:, 6144:7168]], axis=1))
    m["vecs"] = np.ascontiguousarray(np.concatenate(
        [feat_vec(inp["ret_gain"][l]), feat_vec(inp["pool_scale"][l]), feat_vec(inp["ln1_g"][l]), feat_vec(inp["ln1_b"][l]),
         feat_vec(inp["ln2_g"][l]), feat_vec(inp["ln2_b"][l])], axis=1).astype(np.float32))
    m["w_pool"] = np.ascontiguousarray(inp["w_pool"][l].reshape(1024, 256))
    m["w_branch"] = np.ascontiguousarray(inp["w_branch"][l].reshape(3072, D))
    m["w_out"] = np.ascontiguousarray(inp["w_out"][l])
    m["w_r"] = np.ascontiguousarray(np.concatenate([inp["w_r1"][l], inp["w_r2"][l]], axis=1))
    m["b_r"] = np.ascontiguousarray(np.broadcast_to(np.concatenate([inp["b_r1"][l], inp["b_r2"][l]])[None, :], (128, 36)))
    m["w_eg"] = np.ascontiguousarray(inp["w_e_gate"][l].reshape(32 * D, 512))
    m["w_eu"] = np.ascontiguousarray(inp["w_e_up"][l].reshape(32 * D, 512))
    m["w_ed"] = np.ascontiguousarray(inp["w_e_down"][l].reshape(32 * 512, D))
    return m


def gather_payload(outs):
    kT_all = np.concatenate([o["kT_loc"] for o in outs], axis=1)
    v_all = np.concatenate([o["v_loc"].reshape(8, T, 128) for o in outs], axis=1).reshape(8 * SEQ, 128)
    kmean_all = np.concatenate([o["kmean"].reshape(128, 8, 4) for o in outs], axis=2).reshape(128, 256)
    S_all = np.concatenate([o["S_loc"] for o in outs], axis=0)
    res = []
    for c in range(NCORES):
        o = outs[c]
        res.append(dict(kT_all=kT_all, v_all=v_all, kmean_all=kmean_all, S_all=S_all,
                        kT_own=o["kT_loc"], v_own=o["v_loc"], krT2=o["krT2"], vr_tok=o["vr_tok"], kv_loc=o["kv_loc"],
                        halo_in=(outs[c - 1]["halo"] if c > 0 else np.zeros((128, 128), np.float32))))
    return res


_PROGS = {}


def _prog(mode):
    if mode not in _PROGS:
        _PROGS[mode] = build(mode)
    return _PROGS[mode]


def kernel(x, w_in, ret_gain, w_pool, pool_scale, w_branch, w_out, ln1_g, ln1_b,
           w_r1, b_r1, w_r2, b_r2, w_e_gate, w_e_up, w_e_down, ln2_g, ln2_b):
    inp = dict(w_in=w_in, ret_gain=ret_gain, w_pool=w_pool, pool_scale=pool_scale, w_branch=w_branch, w_out=w_out,
               ln1_g=ln1_g, ln1_b=ln1_b, w_r1=w_r1, b_r1=b_r1, w_r2=w_r2, b_r2=b_r2,
               w_e_gate=w_e_gate, w_e_up=w_e_up, w_e_down=w_e_down, ln2_g=ln2_g, ln2_b=ln2_b)
    inp = {k: np.asarray(v, dtype=np.float32) for k, v in inp.items()}
    x = np.asarray(x, dtype=np.float32)
    PA, PLA, PL = _prog("A"), _prog("LA"), _prog("L")
    cores = list(range(NCORES))
    tabs = [host_tables(c) for c in cores]
    xT = [np.ascontiguousarray(x[0, c * T:(c + 1) * T].T) for c in cores]
    A_OUT = ["kT_loc", "v_loc", "kmean", "krT2", "vr_tok", "kv_loc", "S_loc", "halo"]
    li = layer_inputs(inp, 0)
    maps = []
    for c in cores:
        m = {**li, **tabs[c], "xT": xT[c]}
        maps.append({k: m[k] for k in PA.ins})
    ra = run_bass_kernel_spmd(PA.nc, maps, core_ids=cores).results
    for l in range(DEPTH):
        pay = gather_payload(ra)
        last = l == DEPTH - 1
        PX = PL if last else PLA
        nxt = None if last else layer_inputs(inp, l + 1)
        maps = []
        for c in cores:
            m = {**li, **tabs[c], **pay[c], "xT": xT[c]}
            if not last:
                m["w_a"] = nxt["w_a"]
            maps.append({k: m[k] for k in PX.ins})
        rl = run_bass_kernel_spmd(PX.nc, maps, core_ids=cores).results
        xT = [np.asarray(rl[c]["xT_out"], dtype=np.float32) for c in cores]
        if not last:
            ra = [{k: rl[c]["o_" + k] for k in A_OUT} for c in cores]
        li = nxt
        del maps, pay, rl
    out = np.concatenate([xT[c].T for c in cores], axis=0)[None]
    return np.ascontiguousarray(out.astype(np.float32))
```
